# Optimizing a Trainium2 kernel written in Bass

```python
import math
import jax, jax.numpy as jnp
from jax import lax
import numpy as np

D_MODEL = 2048
BATCH = 16
SEQ = 2048
DEPTH = 1

CTX_LEN = 256
GRID_W = 64
D_NA = D_MODEL // 2
HEAD_DIM_NA = 128
N_HEADS_NA = D_NA // HEAD_DIM_NA
WIN_ROWS = 8
WIN_COLS = 16
ROPE_BASE = 10000.0
D_SSM = D_MODEL // 2
SSM_HEAD_DIM = 64
N_HEADS_SSM = D_SSM // SSM_HEAD_DIM
SSM_GROUPS = 2
D_STATE = 128
D_CONV = 5
CHUNK = 128
CONV_DIM = D_SSM + 2 * SSM_GROUPS * D_STATE
D_MIX = D_NA + D_SSM
D_IN_PROJ = 3 * D_NA + D_SSM + CONV_DIM + 2 * N_HEADS_SSM
N_EXPERTS = 16
CAPACITY_FACTOR = 2
D_FF_EXPERT = 2816
EPS = 1e-6

kernel_name = "hybrid_na_ssd_ec_dit_block"

F32 = jnp.float32


def rmsnorm(x, w):
    xf = x.astype(F32)
    y = xf * lax.rsqrt(jnp.mean(xf * xf, axis=-1, keepdims=True) + EPS)
    return (y * w.astype(F32)).astype(x.dtype)


def ada_modulation(cond, w, b):
    mod = jax.nn.silu(cond) @ w + b
    return jnp.split(mod[..., None, :], 6, axis=-1)


def modulate(h, shift, scale):
    return h * (1.0 + scale) + shift


def split_in_proj(p):
    cuts = [D_NA, 2 * D_NA, 3 * D_NA, 3 * D_NA + D_SSM, 3 * D_NA + D_SSM + CONV_DIM]
    return jnp.split(p, cuts, axis=-1)


def axial_rope(x, row_pos, col_pos):
    half = x.shape[-1] // 2
    quarter = half // 2
    freqs = ROPE_BASE ** (-jnp.arange(quarter, dtype=F32) / quarter)

    def rot(u, pos):
        ang = pos.astype(F32)[:, None] * freqs
        cos = jnp.cos(ang)[None, :, None, :]
        sin = jnp.sin(ang)[None, :, None, :]
        u1, u2 = u[..., :quarter], u[..., quarter:]
        return jnp.concatenate([u1 * cos - u2 * sin, u2 * cos + u1 * sin], axis=-1)

    xf = x.astype(F32)
    out = jnp.concatenate([rot(xf[..., :half], row_pos), rot(xf[..., half:], col_pos)], axis=-1)
    return out.astype(x.dtype)


def neighbourhood_attention(q, k, v, k_ctx, v_ctx, rpb):
    nb, t, nh, dh = q.shape
    rows = t // GRID_W
    kh = min(WIN_ROWS, rows)
    kw = min(WIN_COLS, GRID_W)
    scale = dh ** -0.5
    qg = q.reshape(nb, rows, GRID_W, nh, dh)
    kg = k.reshape(nb, rows, GRID_W, nh, dh)
    vg = v.reshape(nb, rows, GRID_W, nh, dh)
    col = jnp.arange(GRID_W)
    col_start = jnp.clip(col - kw // 2, 0, GRID_W - kw)
    col_in = (col[None, :] >= col_start[:, None]) & (col[None, :] < col_start[:, None] + kw)
    col_idx = jnp.clip(col[None, :] - col[:, None] + WIN_COLS - 1, 0, 2 * WIN_COLS - 2)
    rpb_cols = rpb.astype(F32)[:, :, col_idx]

    def row_block(r):
        rs = jnp.clip(r - kh // 2, 0, rows - kh)
        q_r = lax.dynamic_index_in_dim(qg, r, axis=1, keepdims=False)
        k_r = lax.dynamic_slice_in_dim(kg, rs, kh, axis=1)
        v_r = lax.dynamic_slice_in_dim(vg, rs, kh, axis=1)
        row_idx = rs + jnp.arange(kh) - r + WIN_ROWS - 1
        bias = jnp.take(rpb_cols, row_idx, axis=1).transpose(0, 2, 1, 3)
        s_loc = jnp.einsum('bqhd,bkwhd->bhqkw', q_r, k_r).astype(F32) * scale + bias[None]
        s_loc = jnp.where(col_in[:, None, :], s_loc, -jnp.inf)
        s_ctx = jnp.einsum('bqhd,blhd->bhql', q_r, k_ctx).astype(F32) * scale
        s = jnp.concatenate([s_loc.reshape(nb, nh, GRID_W, kh * GRID_W), s_ctx], axis=-1)
        p = jax.nn.softmax(s, axis=-1).astype(v.dtype)
        p_loc = p[..., :kh * GRID_W].reshape(nb, nh, GRID_W, kh, GRID_W)
        p_ctx = p[..., kh * GRID_W:]
        return (jnp.einsum('bhqkw,bkwhd->bqhd', p_loc, v_r)
                + jnp.einsum('bhql,blhd->bqhd', p_ctx, v_ctx))

    out = lax.map(row_block, jnp.arange(rows))
    return jnp.moveaxis(out, 0, 1).reshape(nb, t, nh * dh)


def context_attention(q, k, v):
    nb, l, nh, dh = q.shape
    s = jnp.einsum('bqhd,bkhd->bhqk', q, k).astype(F32) * dh ** -0.5
    p = jax.nn.softmax(s, axis=-1).astype(v.dtype)
    return jnp.einsum('bhqk,bkhd->bqhd', p, v).reshape(nb, l, nh * dh)


def depthwise_conv(u, w, b):
    out = lax.conv_general_dilated(
        u, w.astype(u.dtype)[:, None, :], window_strides=(1,),
        padding=[(D_CONV // 2, D_CONV // 2)],
        dimension_numbers=('NWC', 'WIO', 'NWC'), feature_group_count=u.shape[-1])
    return out + b.astype(u.dtype)


def ssm_inputs(xbc, dt_raw, conv_w, conv_b, dt_bias):
    nb, t = xbc.shape[:2]
    xbc = jax.nn.silu(depthwise_conv(xbc, conv_w, conv_b))
    xs, bm, cm = jnp.split(xbc, [D_SSM, D_SSM + SSM_GROUPS * D_STATE], axis=-1)
    xs = xs.reshape(nb, t, N_HEADS_SSM, SSM_HEAD_DIM)
    bm = bm.reshape(nb, t, SSM_GROUPS, D_STATE)
    cm = cm.reshape(nb, t, SSM_GROUPS, D_STATE)
    dt = jax.nn.softplus(dt_raw.reshape(nb, t, 2, N_HEADS_SSM).astype(F32) + dt_bias.astype(F32))
    return xs, dt, bm, cm


def ssd_chunked(x, dt, A, bm, cm, h0):
    nb, t, nh, p = x.shape
    g, n = bm.shape[-2:]
    e = nh // g
    nc = t // CHUNK
    xf = x.astype(F32).reshape(nb, nc, CHUNK, g, e, p)
    dtc = dt.astype(F32).reshape(nb, nc, CHUNK, g, e)
    a_cs = jnp.cumsum(dtc * A.astype(F32).reshape(g, e), axis=2)
    xdt = xf * dtc[..., None]
    bc = bm.astype(F32).reshape(nb, nc, CHUNK, g, n)
    cc = cm.astype(F32).reshape(nb, nc, CHUNK, g, n)
    lower = jnp.tril(jnp.ones((CHUNK, CHUNK), dtype=bool))
    seg = a_cs[:, :, :, None] - a_cs[:, :, None, :]
    decay_ls = jnp.exp(jnp.where(lower[:, :, None, None], seg, -jnp.inf))
    cb = jnp.einsum('bclgn,bcsgn->bclsg', cc, bc)
    y_diag = jnp.einsum('bclsg,bclsge,bcsgep->bclgep', cb, decay_ls, xdt)
    decay_end = jnp.exp(a_cs[:, :, -1:] - a_cs)
    chunk_states = jnp.einsum('bclgn,bclge,bclgep->bcgepn', bc, decay_end, xdt)
    chunk_decay = jnp.exp(a_cs[:, :, -1])

    def step(h, inp):
        dec, st = inp
        return dec[..., None, None] * h + st, h

    h_last, h_prev = lax.scan(step, h0, (jnp.moveaxis(chunk_decay, 1, 0), jnp.moveaxis(chunk_states, 1, 0)))
    h_prev = jnp.moveaxis(h_prev, 0, 1)
    y_off = jnp.einsum('bclgn,bcgepn,bclge->bclgep', cc, h_prev, jnp.exp(a_cs))
    return (y_diag + y_off).reshape(nb, t, nh, p), h_last


def bidir_ssd(xs, dt, bm, cm, a_log, d_skip, h0_f, h0_b):
    nb, t = xs.shape[:2]
    A = -jnp.exp(a_log.astype(F32))
    flip = lambda u: jnp.flip(u, axis=1)
    y_f, h_f = ssd_chunked(xs, dt[:, :, 0], A[0], bm, cm, h0_f)
    y_b, h_b = ssd_chunked(flip(xs), flip(dt[:, :, 1]), A[1], flip(bm), flip(cm), h0_b)
    y = y_f + flip(y_b) + d_skip.astype(F32)[:, None] * xs.astype(F32)
    return y.astype(xs.dtype).reshape(nb, t, D_SSM), h_f, h_b


def gated_group_rmsnorm(y, z, w):
    nb, t = y.shape[:2]
    u = (y * jax.nn.silu(z)).astype(F32).reshape(nb, t, SSM_GROUPS, D_SSM // SSM_GROUPS)
    u = u * lax.rsqrt(jnp.mean(u * u, axis=-1, keepdims=True) + EPS)
    return (u.reshape(nb, t, D_SSM) * w.astype(F32)).astype(y.dtype)


def expert_choice_ffn(h, w_router, w_gate, w_up, w_down):
    nb, t, d = h.shape
    cap = max(1, CAPACITY_FACTOR * t // N_EXPERTS)
    aff = jax.nn.softmax(jnp.einsum('btd,de->bte', h, w_router).astype(F32), axis=-1)
    gates, idx = lax.top_k(jnp.swapaxes(aff, 1, 2), cap)
    bi = jnp.arange(nb)[:, None, None]
    xs = h[bi, idx]
    hid = jax.nn.silu(jnp.einsum('becd,edf->becf', xs, w_gate)) * jnp.einsum('becd,edf->becf', xs, w_up)
    out = jnp.einsum('becf,efd->becd', hid, w_down) * gates[..., None].astype(h.dtype)
    return jnp.zeros_like(h).at[bi, idx].add(out)


def setup_inputs(seed: int = 0) -> dict:
    key = jax.random.key(seed)
    ks = jax.random.split(key, 24)

    def nrm(k, shape, scale):
        return jax.random.normal(k, shape, F32) * scale

    dt_init = jnp.exp(jax.random.uniform(ks[12], (DEPTH, 2, N_HEADS_SSM), F32,
                                         minval=math.log(1e-3), maxval=math.log(1e-1)))
    return {
        "x": nrm(ks[0], (BATCH, SEQ, D_MODEL), 1.0),
        "c": nrm(ks[1], (BATCH, D_MODEL), 1.0),
        "ctx": nrm(ks[2], (BATCH, CTX_LEN, D_MODEL), 1.0),
        "c_ctx": nrm(ks[3], (D_MODEL,), 1.0),
        "w_ada": nrm(ks[4], (DEPTH, D_MODEL, 6 * D_MODEL), D_MODEL ** -0.5),
        "b_ada": nrm(ks[5], (DEPTH, 6 * D_MODEL), 0.02),
        "norm_mix_w": 1.0 + nrm(ks[6], (DEPTH, D_MODEL), 0.02),
        "norm_ffn_w": 1.0 + nrm(ks[7], (DEPTH, D_MODEL), 0.02),
        "w_in": nrm(ks[8], (DEPTH, D_MODEL, D_IN_PROJ), D_MODEL ** -0.5),
        "rpb": nrm(ks[9], (DEPTH, N_HEADS_NA, 2 * WIN_ROWS - 1, 2 * WIN_COLS - 1), 0.1),
        "conv_w": nrm(ks[10], (DEPTH, D_CONV, CONV_DIM), D_CONV ** -0.5),
        "conv_b": nrm(ks[11], (DEPTH, CONV_DIM), 0.02),
        "dt_bias": dt_init + jnp.log(-jnp.expm1(-dt_init)),
        "a_log": jnp.log(jax.random.uniform(ks[13], (DEPTH, 2, N_HEADS_SSM), F32, minval=1.0, maxval=16.0)),
        "d_skip": 1.0 + nrm(ks[14], (DEPTH, N_HEADS_SSM), 0.02),
        "ssm_norm_w": 1.0 + nrm(ks[15], (DEPTH, D_SSM), 0.02),
        "w_out": nrm(ks[16], (DEPTH, D_MIX, D_MODEL), D_MIX ** -0.5),
        "w_router": nrm(ks[17], (DEPTH, D_MODEL, N_EXPERTS), D_MODEL ** -0.5),
        "w_gate": nrm(ks[18], (DEPTH, N_EXPERTS, D_MODEL, D_FF_EXPERT), D_MODEL ** -0.5),
        "w_up": nrm(ks[19], (DEPTH, N_EXPERTS, D_MODEL, D_FF_EXPERT), D_MODEL ** -0.5),
        "w_down": nrm(ks[20], (DEPTH, N_EXPERTS, D_FF_EXPERT, D_MODEL), D_FF_EXPERT ** -0.5),
        "final_norm_w": 1.0 + nrm(ks[21], (D_MODEL,), 0.02),
    }


def reference(x, c, ctx, c_ctx, w_ada, b_ada, norm_mix_w, norm_ffn_w, w_in, rpb, conv_w, conv_b,
              dt_bias, a_log, d_skip, ssm_norm_w, w_out, w_router, w_gate, w_up, w_down, final_norm_w):
    nb, t, _ = x.shape
    l = ctx.shape[1]
    pos = jnp.arange(t)
    row_pos, col_pos = pos // GRID_W, pos % GRID_W
    h_lat, h_ctx = x, ctx
    for i in range(DEPTH):
        sh1, sc1, g1, sh2, sc2, g2 = ada_modulation(c, w_ada[i], b_ada[i])
        csh1, csc1, cg1, csh2, csc2, cg2 = ada_modulation(c_ctx, w_ada[i], b_ada[i])

        u_lat = modulate(rmsnorm(h_lat, norm_mix_w[i]), sh1, sc1)
        u_ctx = modulate(rmsnorm(h_ctx, norm_mix_w[i]), csh1, csc1)
        q, k, v, z, xbc, dt_raw = split_in_proj(u_lat @ w_in[i])
        qc, kc, vc, zc, xbcc, dtc_raw = split_in_proj(u_ctx @ w_in[i])

        q = axial_rope(q.reshape(nb, t, N_HEADS_NA, HEAD_DIM_NA), row_pos, col_pos)
        k = axial_rope(k.reshape(nb, t, N_HEADS_NA, HEAD_DIM_NA), row_pos, col_pos)
        v = v.reshape(nb, t, N_HEADS_NA, HEAD_DIM_NA)
        kc = kc.reshape(nb, l, N_HEADS_NA, HEAD_DIM_NA)
        vc = vc.reshape(nb, l, N_HEADS_NA, HEAD_DIM_NA)
        attn_lat = neighbourhood_attention(q, k, v, kc, vc, rpb[i])

        h0 = jnp.zeros((nb, SSM_GROUPS, N_HEADS_SSM // SSM_GROUPS, SSM_HEAD_DIM, D_STATE), F32)
        xs_c, dt_c, bm_c, cm_c = ssm_inputs(xbcc, dtc_raw, conv_w[i], conv_b[i], dt_bias[i])
        y_c, hf_c, hb_c = bidir_ssd(xs_c, dt_c, bm_c, cm_c, a_log[i], d_skip[i], h0, h0)
        xs_l, dt_l, bm_l, cm_l = ssm_inputs(xbc, dt_raw, conv_w[i], conv_b[i], dt_bias[i])
        y_l, _, _ = bidir_ssd(xs_l, dt_l, bm_l, cm_l, a_log[i], d_skip[i], hf_c, hb_c)
        ssm_lat = gated_group_rmsnorm(y_l, z, ssm_norm_w[i])

        mix_lat = jnp.concatenate([attn_lat, ssm_lat], axis=-1) @ w_out[i]
        h_lat = h_lat + g1 * mix_lat

        u2 = modulate(rmsnorm(h_lat, norm_ffn_w[i]), sh2, sc2)
        h_lat = h_lat + g2 * expert_choice_ffn(u2, w_router[i], w_gate[i], w_up[i], w_down[i])

        if i < DEPTH - 1:
            attn_ctx = context_attention(qc.reshape(nb, l, N_HEADS_NA, HEAD_DIM_NA), kc, vc)
            ssm_ctx = gated_group_rmsnorm(y_c, zc, ssm_norm_w[i])
            h_ctx = h_ctx + cg1 * (jnp.concatenate([attn_ctx, ssm_ctx], axis=-1) @ w_out[i])
            u2c = modulate(rmsnorm(h_ctx, norm_ffn_w[i]), csh2, csc2)
            h_ctx = h_ctx + cg2 * expert_choice_ffn(u2c, w_router[i], w_gate[i], w_up[i], w_down[i])

    return rmsnorm(h_lat, final_norm_w)
```

```python
import math, os, types
BCUT = int(os.environ.get('BCUT', '9'))
QCUT = int(os.environ.get('QCUT', '9'))
from contextlib import ExitStack
import numpy as np
import concourse.bass as bass
import concourse.mybir as mybir
from concourse.bass_utils import run_bass_kernel_spmd

F32, BF16 = mybir.dt.float32, mybir.dt.bfloat16
ALU = mybir.AluOpType
AF = mybir.ActivationFunctionType

D = 2048; T = 2048; L = 256; NB = 2; KC = 16
DIN = 5664; NH = 8; DH = 128; HS = 16; PS = 64; NG = 2; NS = 128
NE = 16; CAP = 256; FF = 2816; FC = 22
EPS = 1e-6
NEG = -30000.0


class Res:
    __slots__ = ("gen", "w", "r")

    def __init__(self):
        self.gen = -1
        self.w = []
        self.r = []


class Tl:
    def __init__(self, t, excl=False, multi=False):
        self.t = t
        self.res = Res()
        self.excl = excl
        self.multi = multi

    def __getitem__(self, k):
        return self.t[k]


def _freeze(fn):
    if fn.__closure__ is None:
        return fn
    cells = []
    for c in fn.__closure__:
        try:
            cells.append(types.CellType(c.cell_contents))
        except ValueError:
            cells.append(c)
    return types.FunctionType(fn.__code__, fn.__globals__, fn.__name__, fn.__defaults__, tuple(cells))


class Node:
    __slots__ = ("idx", "eng", "fn", "dma", "deps", "succ", "cost", "bytes", "is_out", "tok", "nd")


class Sched:
    def __init__(self, nc, es):
        self.nc = nc
        self.eng = {"pe": nc.tensor, "act": nc.scalar, "dve": nc.vector, "pool": nc.gpsimd, "sp": nc.sync}
        self.sem = {e: es.enter_context(nc.semaphore("s_" + e)) for e in self.eng}
        self.cnt = {e: 0 for e in self.eng}
        self.seen = {e: {} for e in self.eng}
        self.dsem = {}
        for q, n in (("sp", 12), ("pool", 8), ("act", 6)):
            self.dsem[q] = [[es.enter_context(nc.semaphore("d_%s%d" % (q, i))), 0] for i in range(n)]
        self.drr = {q: 0 for q in self.dsem}
        self.out_tokens = []
        self.nodes = []
        self.gen = 0

    def _res(self, t):
        r = t.res
        if r.gen != self.gen:
            r.gen, r.w, r.r = self.gen, [], []
        return r

    def _record(self, nd, reads, writes):
        writes = list(writes) + [r for r in reads if r.excl]
        reads = [r for r in reads if not r.excl]
        deps = set()
        for t in reads:
            deps.update(self._res(t).w)
        for t in writes:
            r = self._res(t)
            if not t.multi:
                deps.update(r.w)
                deps.update(r.r)
        nd.idx = len(self.nodes)
        nd.deps = deps
        nd.succ = []
        nd.tok = None
        self.nodes.append(nd)
        for t in reads:
            r = self._res(t)
            r.r.append(nd.idx)
            if len(r.r) > 64:
                r.r = r.r[-64:]
        for t in writes:
            r = self._res(t)
            if t.multi:
                r.w.append(nd.idx)
            else:
                r.w = [nd.idx]
                r.r = []

    def op(self, e, fn, reads=(), writes=(), cost=0.6):
        nd = Node()
        nd.eng, nd.fn, nd.dma, nd.cost, nd.bytes, nd.is_out = e, _freeze(fn), None, cost, 0, False
        self._record(nd, reads, writes)

    def dma(self, q, out, in_, reads=(), writes=(), is_out=False, nbytes=1 << 20, **kw):
        nd = Node()
        nd.eng, nd.fn, nd.dma, nd.cost, nd.bytes, nd.is_out = q, None, (out, in_, kw), 0.06, nbytes, is_out
        self._record(nd, reads, writes)

    def idma(self, q, fn, reads=(), writes=(), nbytes=1 << 20):
        nd = Node()
        nd.eng, nd.fn, nd.dma, nd.cost, nd.bytes, nd.is_out = q, None, (_freeze(fn),), 0.3, nbytes, False
        self._record(nd, reads, writes)

    def _wait(self, e, tok):
        sem, val = tok
        k = id(sem)
        if self.seen[e].get(k, 0) >= val:
            return
        self.seen[e][k] = val
        self.eng[e].wait_ge(sem, val)

    def flush(self):
        nodes = self.nodes
        n = len(nodes)
        if n == 0:
            return
        for nd in nodes:
            nd.nd = len(nd.deps)
            for d in nd.deps:
                nodes[d].succ.append(nd.idx)
        ready_t = [0.0] * n
        finish = [0.0] * n
        avail = {e: [] for e in self.eng}
        free = {e: 0.0 for e in self.eng}
        for nd in nodes:
            if nd.nd == 0:
                avail[nd.eng].append(nd.idx)
        dma_free = 0.0
        order = []
        left = n
        while left:
            best = None
            for e, lst in avail.items():
                if not lst:
                    continue
                fe = free[e]
                cand = None
                for i in lst:
                    st = ready_t[i] if ready_t[i] > fe else fe
                    key = (st, i)
                    if cand is None or key < cand:
                        cand = key
                if best is None or cand < best[0]:
                    best = (cand, e)
            (st, i), e = best
            avail[e].remove(i)
            nd = nodes[i]
            if nd.dma is not None:
                free[e] = st + nd.cost
                xs = max(st + 0.3, dma_free)
                dma_free = xs + nd.bytes / 250e3
                finish[i] = dma_free + 1.7
            else:
                free[e] = st + nd.cost
                finish[i] = st + nd.cost + 0.15
            order.append(i)
            left -= 1
            for sidx in nd.succ:
                sn = nodes[sidx]
                if finish[i] > ready_t[sidx]:
                    ready_t[sidx] = finish[i]
                sn.nd -= 1
                if sn.nd == 0:
                    avail[sn.eng].append(sidx)
        for i in order:
            nd = nodes[i]
            e = nd.eng
            for d in sorted(nd.deps):
                self._wait(e, nodes[d].tok)
            if nd.dma is not None:
                k = self.drr[e]
                self.drr[e] = (k + 1) % len(self.dsem[e])
                slot = self.dsem[e][k]
                if slot[1]:
                    self._wait(e, (slot[0], slot[1]))
                slot[1] += 16
                if len(nd.dma) == 1:
                    nd.dma[0](self.eng[e]).then_inc(slot[0], 16)
                else:
                    out, in_, kw = nd.dma
                    self.eng[e].dma_start(out=out, in_=in_, **kw).then_inc(slot[0], 16)
                nd.tok = (slot[0], slot[1])
                if nd.is_out:
                    self.out_tokens.append(nd.tok)
            else:
                ins = nd.fn(self.eng[e])
                self.cnt[e] += 1
                ins.then_inc(self.sem[e], 1)
                nd.tok = (self.sem[e], self.cnt[e])
            nd.fn = None
            nd.dma = None
        self.nodes = []
        self.gen += 1

    def barrier(self):
        self.flush()
        toks = [(self.sem[e], self.cnt[e]) for e in self.eng if self.cnt[e]]
        for q in self.dsem:
            toks += [(s[0], s[1]) for s in self.dsem[q] if s[1]]
        for e in self.eng:
            for t in toks:
                self._wait(e, t)

    def finish(self):
        self.flush()
        for t in self.out_tokens:
            self._wait("sp", t)
        self.barrier()


def build(nb=NB, stages="ABCDEFGH", debug=False, kinds="qkvzxd", b1=True):
    nc = bass.Bass("TRN2", target_bir_lowering=False)
    es = ExitStack()
    S = Sched(nc, es)
    okind = "ExternalOutput" if debug else "Internal"

    def din(name, shape, dt=F32):
        return Tl(nc.dram_tensor(name, list(shape), dt, kind="ExternalInput").ap())

    def dscr(name, shape, dt=F32):
        return Tl(nc.dram_tensor(name, list(shape), dt, kind=okind).ap(), multi=True)

    uid = [0]

    def sb(ctx, name, shape, dt=F32):
        uid[0] += 1
        return Tl(ctx.enter_context(nc.sbuf_tensor("%s_%d" % (name, uid[0]), list(shape), dt)))

    def ps(ctx, name, shape=None, dt=F32):
        uid[0] += 1
        return Tl(ctx.enter_context(nc.psum_tensor("%s_%d" % (name, uid[0]), [128, 512] if dt == F32 else [128, 1024], dt)), excl=True)

    xT = din("xT", [nb, D, T])
    ctxT = din("ctxT", [nb, D, L])
    cT = din("cT", [128, KC, 3])
    w_ada = din("w_ada", [D, 6 * D])
    b_adaT = din("b_adaT", [128, 96])
    nmixT = din("nmixT", [128, KC])
    nffnT = din("nffnT", [128, KC])
    finT = din("finT", [128, KC])
    w_in = din("w_in", [D, DIN])
    cos_t = din("cos_t", [128, T])
    sin_t = din("sin_t", [128, T])
    permT = din("permT", [128, 128])
    dtb_bc = din("dtb_bc", [128, 32])
    ident_f = din("ident_f", [128, 128])
    rpbG = din("rpbG", [NH, 128, 35, 128])
    amask = din("amask", [128, 35, 128])
    tri_in = din("tri_in", [2, 128, 128])
    mrep_in = din("mrep_in", [2, 128, 512])
    alog_bc = din("alog_bc", [128, 32])
    dskip_bc = din("dskip_bc", [128, 1024])
    ssmw_bc = din("ssmw_bc", [128, 1024])
    convw = din("convw", [128, 12, 5])
    convb = din("convb", [128, 12])
    w_out = din("w_out", [D, D])
    w_rT = din("w_rT", [128, KC, NE])
    selrow_in = din("selrow_in", [NE, NE, 128])
    iota_in = din("iota_in", [128, 256])
    jcol_in = din("jcol_in", [128, 2])
    tokid_in = din("tokid_in", [128, 16, 2])
    if "G" in stages:
        w_gate = din("w_gate", [NE, D, FF])
        w_up = din("w_up", [NE, D, FF])
        w_down = din("w_down", [NE, FF, D])

    qT_d = dscr("qT_d", [nb, NH, 128, T], BF16)
    kT_d = dscr("kT_d", [nb, NH, 128, T + L], BF16)
    v_d = dscr("v_d", [nb, T + L, NH * DH], BF16)
    sz_d = dscr("sz_d", [nb, T, 1024], F32)
    xbcT_d = dscr("xbcT_d", [nb, 12, 128, T + L], F32)
    dt_d = dscr("dt_d", [nb, T + L, 32], F32)
    mod_d = dscr("mod_d", [128, 96, 3], F32)
    mixT_d = dscr("mixT_d", [nb, 16, 128, T], BF16)
    xs_tok_d = dscr("xs_tok_d", [nb, T + L, 1024], BF16)
    B_tok_d = dscr("B_tok_d", [nb, T + L, 256], BF16)
    BT_d = dscr("BT_d", [nb, 2, 128, T + L], BF16)
    CT_d = dscr("CT_d", [nb, 2, 128, T + L], BF16)
    hT_d = dscr("hT_d", [nb, 2, 16, 128, 1024], BF16)
    h1T_d = dscr("h1T_d", [nb, KC, 128, T], F32)
    u2tok_ds = [dscr("u2tok_d%d" % i, [T, D], BF16) for i in range(nb)]
    pos_d = dscr("pos_d", [nb, NE, T], BF16)
    gate_d = dscr("gate_d", [nb, NE, T], F32)
    posT_d = dscr("posT_d", [nb, 128, 256], F32)
    aff_d = dscr("aff_d", [nb, NE, T], F32)
    XselT_d = dscr("XselT_d", [NE, KC, 128, nb * CAP], BF16)
    Y_d = dscr("Y_d", [nb, NE, CAP, D], BF16)
    outT = Tl(nc.dram_tensor("outT", [nb, D, T], F32, kind="ExternalOutput").ap(), multi=True)

    ones_bf = sb(es, "ones_bf", [128, 128], BF16)
    modT = sb(es, "modT", [128, 96, 3])
    A1 = sb(es, "A1", [128, KC, 3])
    A2 = sb(es, "A2", [128, KC, 3])
    nmix_s = sb(es, "nmix_s", [128, KC])
    nffn_s = sb(es, "nffn_s", [128, KC])
    fin_s = sb(es, "fin_s", [128, KC])
    S.op("dve", lambda e: e.memset(ones_bf[:], 1.0), writes=[ones_bf])
    ones_f = sb(es, "ones_f", [128, 128])
    S.op("dve", lambda e: e.memset(ones_f[:], 1.0), writes=[ones_f])
    idf_s = sb(es, "idf_s", [128, 128])
    id_bf = sb(es, "id_bf", [128, 128], BF16)
    S.dma("sp", idf_s[:], ident_f[:], reads=[ident_f], writes=[idf_s])
    S.op("dve", lambda e: e.tensor_copy(out=id_bf[:], in_=idf_s[:]), reads=[idf_s], writes=[id_bf])
    S.dma("sp", nmix_s[:], nmixT[:], reads=[nmixT], writes=[nmix_s])
    S.dma("sp", nffn_s[:], nffnT[:], reads=[nffnT], writes=[nffn_s])
    S.dma("sp", fin_s[:], finT[:], reads=[finT], writes=[fin_s])

    c_s = sb(es, "c_s", [128, KC, 3])
    sc_bf = sb(es, "sc_bf", [128, KC, 3], BF16)
    bada_s = sb(es, "bada_s", [128, 96])

    def ada(cx, groups):
        wa = [sb(cx, "wa%d" % i, [128, KC, 1024], BF16) for i in range(2)]
        pa = [ps(cx, "pa%d" % i) for i in range(2)]
        wv = w_ada.t.rearrange("(kc p) n -> p kc n", p=128)
        for g in groups:
            w = wa[g % 2]
            p_ = pa[g % 2]
            S.dma("pool", w[:], wv[:, :, g * 1024:(g + 1) * 1024], reads=[w_ada], writes=[w], nbytes=128 * KC * 1024 * 4)

            def mm(e, w=w, p_=p_):
                ins = None
                for j in range(8):
                    for kc in range(KC):
                        ins = e.matmul(p_[:, j * 4:j * 4 + 3], lhsT=w[:, kc, j * 128:(j + 1) * 128], rhs=sc_bf[:, kc, :],
                                       start=(kc == 0), stop=(kc == KC - 1))
                return ins
            S.op("pe", mm, reads=[w, sc_bf], writes=[p_], cost=10.0)
            S.op("dve", lambda e, g=g, p_=p_: e.tensor_tensor(
                out=modT[:, g * 8:(g + 1) * 8, :], in0=p_[:, 0:32].rearrange("p (j r) -> p j r", r=4)[:, :, 0:3],
                in1=bada_s[:, g * 8:(g + 1) * 8].unsqueeze(2).to_broadcast([128, 8, 3]), op=ALU.add),
                reads=[p_, bada_s], writes=[modT2 if g >= 4 else modT])

    def ada_fin(A_, n_, off):
        S.op("dve", lambda e: e.scalar_tensor_tensor(
            out=A_[:], in0=modT[:, off:off + 16, :], scalar=1.0,
            in1=n_[:].unsqueeze(2).to_broadcast([128, KC, 3]), op0=ALU.add, op1=ALU.mult),
            reads=[modT2 if off >= 32 else modT, n_], writes=[A_])

    modT2 = Tl(modT.t)
    if "A" in stages:
        with ExitStack() as cx:
            S.dma("sp", c_s[:], cT[:], reads=[cT], writes=[c_s])
            S.dma("sp", bada_s[:], b_adaT[:], reads=[b_adaT], writes=[bada_s])
            S.op("act", lambda e: e.activation(out=sc_bf[:], in_=c_s[:], func=AF.Silu), reads=[c_s], writes=[sc_bf])
            ada(cx, range(4))
            ada_fin(A1, nmix_s, 16)
            S.barrier()

    SH1, G1, SH2, G2 = 0, 32, 48, 80

    def stage_B(b):
        with ExitStack() as cx:
            uT_lat = [sb(cx, "uT%d" % i, [128, KC, 512], BF16) for i in range(4)]
            uT_ctx = [sb(cx, "uTc", [128, KC, L], BF16)]
            xs = [sb(cx, "xs%d" % i, [128, KC, 256]) for i in range(2)]
            sq = [sb(cx, "sq%d" % i, [128, KC, 256], BF16) for i in range(2)]
            rstd = [sb(cx, "rstd%d" % i, [128, 256]) for i in range(2)]
            tmp = [sb(cx, "tmpB%d" % i, [128, 512]) for i in range(3)]
            wi = [sb(cx, "wi%d" % i, [128, KC, 512], BF16) for i in range(2)]
            cos_s = sb(cx, "cos_s", [128, T])
            sin_s = sb(cx, "sin_s", [128, T])
            perm_f = sb(cx, "perm_f", [128, 128])
            perm_s = sb(cx, "perm_s", [128, 128], BF16)
            dtb_s = sb(cx, "dtb_s", [128, 32])
            qsb = [sb(cx, "qsb%d" % i, [128, 512], BF16) for i in range(2)]
            ost = [sb(cx, "ost%d" % i, [128, 512]) for i in range(3)]
            ostb = [sb(cx, "ostb%d" % i, [128, 512], BF16) for i in range(3)]
            pss = ps(cx, "pss", [128, 256])
            pm = [ps(cx, "pm%d" % i, [128, 512]) for i in range(4)]
            pw = [ps(cx, "pw%d" % i, [128, 512]) for i in range(2)]
            S.dma("sp", cos_s[:], cos_t[:], reads=[cos_t], writes=[cos_s])
            S.dma("sp", sin_s[:], sin_t[:], reads=[sin_t], writes=[sin_s])
            S.dma("sp", perm_f[:], permT[:], reads=[permT], writes=[perm_f])
            S.dma("sp", dtb_s[:], dtb_bc[:], reads=[dtb_bc], writes=[dtb_s])
            S.op("dve", lambda e: e.tensor_copy(out=perm_s[:], in_=perm_f[:]), reads=[perm_f], writes=[perm_s])
            wv = w_in.t.rearrange("(kc p) n -> p kc n", p=128)
            cnt = {"x": 0, "pm": 0, "pw": 0, "ost": 0, "w": 0, "tmp": 0, "q": 0}

            for (src, ntok, r, tok0) in ((ctxT, L, 2, T), (xT, T, b, 0)):
                is_ctx = (r == 2)
                uT = uT_ctx if is_ctx else uT_lat
                sv = src.t[b].rearrange("(kc p) t -> p kc t", p=128)
                for t0 in (range(0, ntok, 256) if b1 else []):
                    i = cnt["x"] % 2
                    cnt["x"] += 1
                    x_, s_, r_ = xs[i], sq[i], rstd[i]
                    S.dma("sp", x_[:], sv[:, :, t0:t0 + 256], reads=[src], writes=[x_])
                    if BCUT < 1: continue
                    S.op("act", lambda e, x_=x_, s_=s_: e.activation(out=s_[:], in_=x_[:], func=AF.Square),
                         reads=[x_], writes=[s_])
                    if BCUT < 2: continue

                    def mmss(e, s_=s_):
                        ins = None
                        for kc in range(KC):
                            ins = e.matmul(pss[:, 0:256], lhsT=ones_bf[:], rhs=s_[:, kc, :], start=(kc == 0), stop=(kc == KC - 1))
                        return ins
                    S.op("pe", mmss, reads=[s_, ones_bf], writes=[pss])
                    if BCUT < 3: continue
                    S.op("dve", lambda e, r_=r_: e.tensor_scalar(out=r_[:], in0=pss[:, 0:256], scalar1=1.0 / D, scalar2=EPS,
                                                                  op0=ALU.mult, op1=ALU.add), reads=[pss], writes=[r_])
                    S.op("act", lambda e, r_=r_: e.activation(out=r_[:], in_=r_[:], func=AF.Sqrt), reads=[r_], writes=[r_])
                    S.op("dve", lambda e, r_=r_: e.reciprocal(out=r_[:], in_=r_[:]), reads=[r_], writes=[r_])
                    for kc in range(KC if BCUT >= 4 else 0):
                        tm = tmp[cnt["tmp"] % 3]
                        cnt["tmp"] += 1
                        S.op("dve", lambda e, x_=x_, r_=r_, tm=tm, kc=kc: e.scalar_tensor_tensor(
                            out=tm[:, 0:256], in0=x_[:, kc, :], scalar=A1[:, kc, r:r + 1], in1=r_[:],
                            op0=ALU.mult, op1=ALU.mult), reads=[x_, r_, A1], writes=[tm])
                        uq = uT[t0 // 512]
                        S.op("act", lambda e, tm=tm, kc=kc, t0=t0, uq=uq: e.activation(
                            out=uq[:, kc, t0 % 512:t0 % 512 + 256], in_=tm[:, 0:256], func=AF.Identity,
                            bias=modT[:, SH1 + kc, r:r + 1]), reads=[tm, modT], writes=[uq])

                tts = [(t0, min(512, ntok - t0)) for t0 in range(0, ntok, 512)]
                for g in range(12):
                    kind = ("q", "q", "k", "k", "v", "v", "z", "z", "x", "x", "x", "d")[g]
                    if (is_ctx and kind in ("q", "z")) or kind not in kinds:
                        continue
                    ncol = 512 if g < 11 else 32
                    w = wi[cnt["w"] % 2]
                    cnt["w"] += 1
                    S.dma("pool", w[:, :, 0:ncol], wv[:, :, g * 512:g * 512 + ncol], reads=[w_in], writes=[w])
                    if kind in ("v", "d", "z"):
                        for tc_ in range(ntok // 128):
                            p_ = pm[cnt["pm"] % 4]
                            cnt["pm"] += 1

                            uq = uT[tc_ // 4]

                            def mmv(e, w=w, p_=p_, tc_=tc_, ncol=ncol, uq=uq):
                                ins = None
                                for kc in range(KC):
                                    ins = e.matmul(p_[:, 0:ncol], lhsT=uq[:, kc, (tc_ % 4) * 128:(tc_ % 4 + 1) * 128],
                                                   rhs=w[:, kc, 0:ncol], start=(kc == 0), stop=(kc == KC - 1))
                                return ins
                            S.op("pe", mmv, reads=[w, uq], writes=[p_], cost=16 * max(ncol, 64) / 2200.0)
                            row0 = tok0 + tc_ * 128
                            if kind == "z":
                                o_ = ost[cnt["ost"] % 3]
                                cnt["ost"] += 1
                                S.op("act", lambda e, o_=o_, p_=p_: e.activation(out=o_[:], in_=p_[:], func=AF.Silu),
                                     reads=[p_], writes=[o_])
                                c0 = (g - 6) * 512
                                S.dma("sp", sz_d.t[b, row0:row0 + 128, c0:c0 + 512], o_[:], reads=[o_], writes=[sz_d])
                            elif kind == "v":
                                o_ = ostb[cnt["ost"] % 3]
                                cnt["ost"] += 1
                                S.op("act", lambda e, o_=o_, p_=p_: e.activation(out=o_[:], in_=p_[:], func=AF.Copy),
                                     reads=[p_], writes=[o_])
                                c0 = (g - 4) * 512
                                S.dma("sp", v_d.t[b, row0:row0 + 128, c0:c0 + 512], o_[:], reads=[o_], writes=[v_d])
                            else:
                                o_ = ost[cnt["ost"] % 3]
                                cnt["ost"] += 1
                                S.op("dve", lambda e, o_=o_, p_=p_: e.tensor_tensor(out=o_[:, 0:32], in0=p_[:, 0:32],
                                                                                   in1=dtb_s[:], op=ALU.add),
                                     reads=[p_, dtb_s], writes=[o_])
                                S.op("act", lambda e, o_=o_: e.activation(out=o_[:, 0:32], in_=o_[:, 0:32], func=AF.Exp),
                                     reads=[o_], writes=[o_])
                                S.op("act", lambda e, o_=o_: e.activation(out=o_[:, 0:32], in_=o_[:, 0:32], func=AF.Ln,
                                                                          bias=1.0), reads=[o_], writes=[o_])
                                S.dma("sp", dt_d.t[b, row0:row0 + 128, :], o_[:, 0:32], reads=[o_], writes=[dt_d])
                        continue
                    for j in range(4):
                        ch = g * 4 + j
                        for (t0, tn) in tts:
                            p_ = pm[cnt["pm"] % 4]
                            cnt["pm"] += 1

                            uq = uT[t0 // 512]

                            def mmf(e, w=w, p_=p_, j=j, t0=t0, tn=tn, uq=uq):
                                ins = None
                                for kc in range(KC):
                                    ins = e.matmul(p_[:, 0:tn], lhsT=w[:, kc, j * 128:(j + 1) * 128],
                                                   rhs=uq[:, kc, 0:tn], start=(kc == 0), stop=(kc == KC - 1))
                                return ins
                            S.op("pe", mmf, reads=[w, uq], writes=[p_], cost=16 * tn / 2200.0)
                            if kind in ("q", "k") and not is_ctx:
                                qb = qsb[cnt["q"] % 2]
                                p2 = pw[cnt["q"] % 2]
                                cnt["q"] += 1
                                S.op("act", lambda e, qb=qb, p_=p_: e.activation(out=qb[:], in_=p_[:], func=AF.Copy),
                                     reads=[p_], writes=[qb])
                                if QCUT < 2: continue
                                S.op("pe", lambda e, qb=qb, p2=p2: e.matmul(p2[:], lhsT=perm_s[:], rhs=qb[:],
                                                                              start=True, stop=True),
                                     reads=[qb, perm_s], writes=[p2])
                                if QCUT < 3: continue
                                ta = tmp[cnt["tmp"] % 3]
                                tb = tmp[(cnt["tmp"] + 1) % 3]
                                cnt["tmp"] += 2
                                S.op("dve", lambda e, ta=ta, p_=p_, t0=t0: e.tensor_tensor(
                                    out=ta[:], in0=p_[:], in1=cos_s[:, t0:t0 + 512], op=ALU.mult),
                                    reads=[p_, cos_s], writes=[ta])
                                S.op("dve", lambda e, tb=tb, p2=p2, t0=t0: e.tensor_tensor(
                                    out=tb[:], in0=p2[:], in1=sin_s[:, t0:t0 + 512], op=ALU.mult),
                                    reads=[p2, sin_s], writes=[tb])
                                if QCUT < 4: continue
                                o_ = ostb[cnt["ost"] % 3]
                                cnt["ost"] += 1
                                S.op("dve", lambda e, o_=o_, ta=ta, tb=tb: e.tensor_tensor(
                                    out=o_[:], in0=ta[:], in1=tb[:], op=ALU.add), reads=[ta, tb], writes=[o_])
                                if QCUT < 5: continue
                                dst = qT_d if kind == "q" else kT_d
                                h = ch % 8
                                S.dma("sp", dst.t[b, h, :, t0:t0 + 512], o_[:], reads=[o_], writes=[dst])
                            elif kind == "k":
                                o_ = ostb[cnt["ost"] % 3]
                                cnt["ost"] += 1
                                S.op("act", lambda e, o_=o_, p_=p_, tn=tn: e.activation(out=o_[:, 0:tn], in_=p_[:, 0:tn],
                                                                                        func=AF.Copy),
                                     reads=[p_], writes=[o_])
                                S.dma("sp", kT_d.t[b, ch % 8, :, T + t0:T + t0 + tn], o_[:, 0:tn], reads=[o_], writes=[kT_d])
                            else:
                                o_ = ost[cnt["ost"] % 3]
                                cnt["ost"] += 1
                                S.op("act", lambda e, o_=o_, p_=p_, tn=tn: e.activation(out=o_[:, 0:tn], in_=p_[:, 0:tn],
                                                                                        func=AF.Copy),
                                     reads=[p_], writes=[o_])
                                S.dma("sp", xbcT_d.t[b, ch - 32, :, tok0 + t0:tok0 + t0 + tn], o_[:, 0:tn],
                                      reads=[o_], writes=[xbcT_d])
            S.barrier()


    def stage_C(b):
        scale = DH ** -0.5
        with ExitStack() as cx:
            am_s = sb(cx, "am_s", [128, 35, 128])
            bias = [sb(cx, "bias%d" % i, [128, 35, 128]) for i in range(2)]
            biasb = [sb(cx, "biasb%d" % i, [128, 35, 128], BF16) for i in range(2)]
            qh = [sb(cx, "qh%d" % i, [128, T], BF16) for i in range(2)]
            kh = [sb(cx, "kh%d" % i, [128, T + L], BF16) for i in range(2)]
            vh = [sb(cx, "vh%d" % i, [128, 18, 128], BF16) for i in range(2)]
            oh = [sb(cx, "oh%d" % i, [128, T], BF16) for i in range(2)]
            E_ = [sb(cx, "E%d" % i, [128, 7, 128]) for i in range(2)]
            P_ = [sb(cx, "P%d" % i, [128, 7, 128], BF16) for i in range(2)]
            rd = [sb(cx, "rd%d" % i, [128, 128]) for i in range(2)]
            pSa = [ps(cx, "pSa%d" % i) for i in range(2)]
            pSb = [ps(cx, "pSb%d" % i) for i in range(2)]
            pden = [ps(cx, "pden%d" % i) for i in range(2)]
            pO = [ps(cx, "pO%d" % i) for i in range(2)]
            S.dma("sp", am_s[:], amask[:], reads=[amask], writes=[am_s])
            it = 0
            for h in range(NH):
                i = h % 2
                bi, q_, k_, v_, o_ = bias[i], qh[i], kh[i], vh[i], oh[i]
                bb_ = biasb[i]
                S.dma("sp", bi[:], rpbG.t[h], reads=[rpbG], writes=[bi])
                S.dma("sp", q_[:], qT_d.t[b, h], reads=[qT_d], writes=[q_])
                S.dma("sp", k_[:], kT_d.t[b, h], reads=[kT_d], writes=[k_])
                S.dma("sp", v_[:], v_d.t[b].rearrange("(c p) n -> p c n", p=128)[:, :, h * 128:(h + 1) * 128],
                      reads=[v_d], writes=[v_])
                S.op("dve", lambda e, bi=bi, bb_=bb_: e.scalar_tensor_tensor(
                    out=bb_[:], in0=bi[:], scalar=1.0, in1=am_s[:], op0=ALU.mult, op1=ALU.add),
                    reads=[am_s, bi], writes=[bb_], cost=4.7)
                S.op("dve", lambda e, bb_=bb_: e.tensor_scalar(out=bb_[:], in0=bb_[:], scalar1=1.0 / scale, scalar2=None,
                                                               op0=ALU.mult), reads=[], writes=[bb_], cost=2.4)
                for rp in range(16):
                    case = {0: 0, 1: 1, 14: 3, 15: 4}.get(rp, 2)
                    ks = min(max(2 * rp - 4, 0), 22) * 64
                    kofs = [ks + j * 128 for j in range(5)] + [T, T + 128]
                    j2 = it % 2
                    it += 1
                    a_, b_, d_, O_, e_, p_, r_ = pSa[j2], pSb[j2], pden[j2], pO[j2], E_[j2], P_[j2], rd[j2]

                    def mms(e, a_=a_, b_=b_, k_=k_, q_=q_, rp=rp, kofs=kofs, bb_=bb_, case=case):
                        ins = None
                        for j in range(7):
                            dst = a_[:, j * 128:(j + 1) * 128] if j < 4 else b_[:, (j - 4) * 128:(j - 3) * 128]
                            ins = e.matmul(dst, lhsT=k_[:, kofs[j]:kofs[j] + 128], rhs=q_[:, rp * 128:(rp + 1) * 128],
                                           start=True, stop=(j >= 5))
                            if j < 5:
                                ins = e.matmul(dst, lhsT=id_bf[:], rhs=bb_[:, case * 7 + j, :], start=False, stop=True)
                        return ins
                    S.op("pe", mms, reads=[k_, q_, bb_, id_bf], writes=[a_, b_], cost=1.0)
                    S.op("act", lambda e, a_=a_, p_=p_: e.activation(
                        out=p_[:, 0:4, :], in_=a_[:, 0:512].rearrange("p (j q) -> p j q", q=128), func=AF.Exp, scale=scale),
                        reads=[a_], writes=[p_], cost=0.6)
                    S.op("act", lambda e, b_=b_, p_=p_: e.activation(
                        out=p_[:, 4:7, :], in_=b_[:, 0:384].rearrange("p (j q) -> p j q", q=128), func=AF.Exp, scale=scale),
                        reads=[b_], writes=[p_], cost=0.5)

                    def mmo(e, d_=d_, O_=O_, p_=p_, v_=v_, kofs=kofs):
                        ins = None
                        for j in range(7):
                            e.matmul(d_[:, 0:128], lhsT=ones_bf[:], rhs=p_[:, j, :], start=(j == 0), stop=(j == 6))
                        for j in range(7):
                            ins = e.matmul(O_[:, 0:128], lhsT=v_[:, kofs[j] // 128, :], rhs=p_[:, j, :],
                                           start=(j == 0), stop=(j == 6))
                        return ins
                    S.op("pe", mmo, reads=[p_, v_, ones_bf], writes=[d_, O_])
                    S.op("dve", lambda e, d_=d_, r_=r_: e.reciprocal(out=r_[:], in_=d_[:, 0:128]), reads=[d_], writes=[r_])
                    S.op("dve", lambda e, O_=O_, r_=r_, o_=o_, rp=rp: e.tensor_tensor(
                        out=o_[:, rp * 128:(rp + 1) * 128], in0=O_[:, 0:128], in1=r_[:], op=ALU.mult),
                        reads=[O_, r_], writes=[o_])
                S.dma("sp", mixT_d.t[b, h], o_[:], reads=[o_], writes=[mixT_d])
            S.barrier()

    def stage_D1(b):
        TL = T + L
        with ExitStack() as cx:
            cw = sb(cx, "cw", [128, 12, 5])
            cb = sb(cx, "cb", [128, 12])
            xr = [sb(cx, "xr%d" % i, [128, TL]) for i in range(2)]
            xb = [sb(cx, "xb%d" % i, [128, TL], BF16) for i in range(2)]
            dg = [sb(cx, "dg%d" % i, [128, 5, 128], BF16) for i in range(2)]
            so = [sb(cx, "so%d" % i, [128, TL], BF16) for i in range(2)]
            tr = [sb(cx, "tr%d" % i, [128, 8, 128], BF16) for i in range(2)]
            pt = [ps(cx, "pt%d" % i, dt=BF16) for i in range(2)]
            pcv = [ps(cx, "pcv%d" % i) for i in range(3)]
            if b == 0 and "A" in stages:
                ada(cx, range(4, 12))
                ada_fin(A2, nffn_s, 64)
                if debug:
                    S.dma("sp", mod_d[:], modT[:], reads=[modT, modT2], writes=[mod_d], is_out=True)
            S.dma("sp", cw[:], convw[:], reads=[convw], writes=[cw])
            S.dma("sp", cb[:], convb[:], reads=[convb], writes=[cb])
            nt = 0
            ncv = 0
            for ch in range(12):
                x_, xb_, dg_, s_ = xr[ch % 2], xb[ch % 2], dg[ch % 2], so[ch % 2]
                S.dma("sp" if ch % 2 else "act", x_[:], xbcT_d.t[b, ch], reads=[xbcT_d], writes=[x_])
                S.op("dve", lambda e, x_=x_, xb_=xb_: e.tensor_copy(out=xb_[:], in_=x_[:]), reads=[x_], writes=[xb_], cost=1.5)

                def mkdg(e, dg_=dg_, ch=ch):
                    ins = None
                    for j in range(5):
                        ins = e.tensor_scalar(out=dg_[:, j, :], in0=idf_s[:], scalar1=cw[:, ch, j:j + 1], scalar2=None, op0=ALU.mult)
                    return ins
                S.op("dve", mkdg, reads=[idf_s, cw], writes=[dg_], cost=1.0)
                for c0 in range(0, TL, 512):
                    w_ = min(512, TL - c0)
                    s0, s1 = (0, T) if c0 < T else (T, TL)
                    pc = pcv[ncv % 3]
                    ncv += 1

                    def mmc(e, pc=pc, dg_=dg_, xb_=xb_, c0=c0, w_=w_, s0=s0, s1=s1):
                        e.matmul(pc[:, 0:w_], lhsT=dg_[:, 2, :], rhs=xb_[:, c0:c0 + w_], start=True, stop=False)
                        ins = None
                        for jj, j in enumerate((0, 1, 3, 4)):
                            off = j - 2
                            lo, hi = max(c0, s0 - off), min(c0 + w_, s1 - off)
                            ins = e.matmul(pc[:, lo - c0:hi - c0], lhsT=dg_[:, j, :], rhs=xb_[:, lo + off:hi + off],
                                           start=False, stop=(jj == 3))
                        return ins
                    S.op("pe", mmc, reads=[dg_, xb_], writes=[pc], cost=1.2)
                    S.op("act", lambda e, pc=pc, s_=s_, c0=c0, w_=w_, ch=ch: e.activation(
                        out=s_[:, c0:c0 + w_], in_=pc[:, 0:w_], func=AF.Silu, bias=cb[:, ch:ch + 1]),
                        reads=[pc, cb], writes=[s_], cost=0.6)
                if ch >= 8:
                    dst = BT_d if ch < 10 else CT_d
                    S.dma("sp", dst.t[b, ch % 2], s_[:], reads=[s_], writes=[dst])
                if ch < 10:
                    for c8 in range(0, 18, 8):
                        n = min(8, 18 - c8)
                        p_, t_ = pt[nt % 2], tr[nt % 2]
                        nt += 1

                        def tp(e, p_=p_, s_=s_, c8=c8, n=n):
                            ins = None
                            for i in range(n):
                                ins = e.transpose(p_[:, i * 128:(i + 1) * 128], s_[:, (c8 + i) * 128:(c8 + i + 1) * 128], id_bf[:])
                            return ins
                        S.op("pe", tp, reads=[s_, id_bf], writes=[p_])
                        S.op("act", lambda e, p_=p_, t_=t_, n=n: e.activation(
                            out=t_[:, 0:n, :], in_=p_[:, 0:n * 128].rearrange("p (i c) -> p i c", c=128), func=AF.Copy),
                            reads=[p_], writes=[t_])
                        if ch < 8:
                            dv = xs_tok_d.t[b, c8 * 128:(c8 + n) * 128, ch * 128:(ch + 1) * 128]
                            dres = xs_tok_d
                        else:
                            dv = B_tok_d.t[b, c8 * 128:(c8 + n) * 128, (ch - 8) * 128:(ch - 7) * 128]
                            dres = B_tok_d
                        S.dma("sp", dv.rearrange("(i p) n -> p i n", p=128), t_[:, 0:n, :], reads=[t_], writes=[dres])
            S.barrier()

    def stage_D2(b):
        with ExitStack() as cx:
            xs_t = sb(cx, "xs_t", [128, 18, 1024], BF16)
            dt_t = sb(cx, "dt_t", [128, 18, 32])
            a_t = sb(cx, "a_t", [128, 18, 32])
            Bt = sb(cx, "Bt", [128, 18, 256], BF16)
            BT = sb(cx, "BT", [128, 2, T + L], BF16)
            CT = sb(cx, "CT", [128, 2, T + L], BF16)
            tri = sb(cx, "tri", [128, 2, 128])
            mrep = sb(cx, "mrep", [128, 2, 512])
            Abc = sb(cx, "Abc", [128, 32])
            dsk = sb(cx, "dsk", [128, 1024])
            S.dma("sp", xs_t[:], xs_tok_d.t[b].rearrange("(c p) n -> p c n", p=128), reads=[xs_tok_d], writes=[xs_t])
            S.dma("sp", dt_t[:], dt_d.t[b].rearrange("(c p) n -> p c n", p=128), reads=[dt_d], writes=[dt_t])
            S.dma("sp", Bt[:], B_tok_d.t[b].rearrange("(c p) n -> p c n", p=128), reads=[B_tok_d], writes=[Bt])
            S.dma("sp", BT[:], BT_d.t[b].rearrange("g p t -> p g t"), reads=[BT_d], writes=[BT])
            S.dma("sp", CT[:], CT_d.t[b].rearrange("g p t -> p g t"), reads=[CT_d], writes=[CT])
            S.dma("sp", tri[:], tri_in.t.rearrange("d p l -> p d l"), reads=[tri_in], writes=[tri])
            S.dma("sp", mrep[:], mrep_in.t.rearrange("d p l -> p d l"), reads=[mrep_in], writes=[mrep])
            S.dma("sp", Abc[:], alog_bc[:], reads=[alog_bc], writes=[Abc])
            S.dma("sp", dsk[:], dskip_bc[:], reads=[dskip_bc], writes=[dsk])
            S.op("act", lambda e: e.activation(out=Abc[:], in_=Abc[:], func=AF.Exp), reads=[Abc], writes=[Abc])
            S.op("dve", lambda e: e.scalar_tensor_tensor(
                out=a_t[:], in0=dt_t[:], scalar=-1.0, in1=Abc[:].unsqueeze(1).to_broadcast([128, 18, 32]),
                op0=ALU.mult, op1=ALU.mult), reads=[dt_t, Abc], writes=[a_t])

            with ExitStack() as c1:
                hst = [sb(c1, "hst%d" % d, [128, 1024]) for d in range(2)]
                hbf = [sb(c1, "hbf%d" % i, [128, 1024], BF16) for i in range(2)]
                acs = [sb(c1, "acs%d" % i, [128, 32]) for i in range(2)]
                wd = [sb(c1, "wd%d" % i, [128, 32]) for i in range(2)]
                cdb = [sb(c1, "cdb%d" % i, [128, 32]) for i in range(2)]
                xw = [sb(c1, "xw%d" % i, [128, 1024], BF16) for i in range(2)]
                tmpS = [sb(c1, "tmpS%d" % i, [128, 1024]) for i in range(2)]
                pc = [ps(c1, "pc%d" % i) for i in range(2)]
                pi = [ps(c1, "pi%d" % i) for i in range(4)]
                n1 = 0
                for d in range(2):
                    S.op("dve", lambda e, d=d: e.memset(hst[d][:], 0.0), writes=[hst[d]])
                    order = [16, 17] + list(range(16)) if d == 0 else [17, 16] + list(range(15, -1, -1))
                    for c in order:
                        k = n1 % 2
                        n1 += 1
                        ac, w_, cd_, xw_, pc_, tm = acs[k], wd[k], cdb[k], xw[k], pc[k], tmpS[k]
                        if c < 16:
                            hb = hbf[k]
                            S.op("act", lambda e, hb=hb, d=d: e.activation(out=hb[:], in_=hst[d][:], func=AF.Copy),
                                 reads=[hst[d]], writes=[hb])
                            S.dma("sp", hT_d.t[b, d, c], hb[:], reads=[hb], writes=[hT_d])

                        def mmc(e, pc_=pc_, c=c, d=d):
                            e.matmul(pc_[:, 0:16], lhsT=tri[:, d, :], rhs=a_t[:, c, d * 16:(d + 1) * 16], start=True, stop=True)
                            return e.matmul(pc_[:, 32:48], lhsT=ones_f[:], rhs=a_t[:, c, d * 16:(d + 1) * 16],
                                            start=True, stop=True)
                        S.op("pe", mmc, reads=[tri, a_t, ones_f], writes=[pc_])
                        S.op("act", lambda e, pc_=pc_, ac=ac: e.activation(out=ac[:, 0:16], in_=pc_[:, 0:16], func=AF.Copy),
                             reads=[pc_], writes=[ac])
                        S.op("dve", lambda e, pc_=pc_, w_=w_, ac=ac: e.tensor_tensor(out=w_[:, 0:16], in0=pc_[:, 32:48],
                                                                                      in1=ac[:, 0:16], op=ALU.subtract),
                             reads=[pc_, ac], writes=[w_])
                        S.op("act", lambda e, w_=w_: e.activation(out=w_[:, 0:16], in_=w_[:, 0:16], func=AF.Exp),
                             reads=[w_], writes=[w_])
                        S.op("act", lambda e, pc_=pc_, cd_=cd_: e.activation(out=cd_[:, 0:16], in_=pc_[:, 32:48], func=AF.Exp),
                             reads=[pc_], writes=[cd_])
                        S.op("dve", lambda e, w_=w_, c=c, d=d: e.tensor_tensor(
                            out=w_[:, 0:16], in0=w_[:, 0:16], in1=dt_t[:, c, d * 16:(d + 1) * 16], op=ALU.mult),
                            reads=[dt_t], writes=[w_])
                        S.op("dve", lambda e, w_=w_, xw_=xw_, c=c: e.tensor_tensor(
                            out=xw_[:].rearrange("p (h q) -> p h q", q=64), in0=xs_t[:, c, :].rearrange("p (h q) -> p h q", q=64),
                            in1=w_[:, 0:16].unsqueeze(2).to_broadcast([128, 16, 64]), op=ALU.mult),
                            reads=[xs_t, w_], writes=[xw_])
                        for g in range(2):
                            pi_ = pi[(n1 * 2 + g) % 4]
                            S.op("pe", lambda e, pi_=pi_, c=c, g=g, xw_=xw_: e.matmul(
                                pi_[:], lhsT=Bt[:, c, g * 128:(g + 1) * 128], rhs=xw_[:, g * 512:(g + 1) * 512],
                                start=True, stop=True), reads=[Bt, xw_], writes=[pi_])
                            S.op("dve", lambda e, tm=tm, d=d, g=g, cd_=cd_: e.tensor_tensor(
                                out=tm[:, g * 512:(g + 1) * 512].rearrange("p (h q) -> p h q", q=64),
                                in0=hst[d][:, g * 512:(g + 1) * 512].rearrange("p (h q) -> p h q", q=64),
                                in1=cd_[:, g * 8:(g + 1) * 8].unsqueeze(2).to_broadcast([128, 8, 64]), op=ALU.mult),
                                reads=[hst[d], cd_], writes=[tm])
                            S.op("dve", lambda e, tm=tm, d=d, g=g, pi_=pi_: e.tensor_tensor(
                                out=hst[d][:, g * 512:(g + 1) * 512], in0=pi_[:], in1=tm[:, g * 512:(g + 1) * 512], op=ALU.add),
                                reads=[pi_, tm], writes=[hst[d]])
                S.barrier()

            with ExitStack() as c2:
                wn = sb(c2, "wn", [128, 1024])
                R_s = [sb(c2, "R_%d" % i, [128, 16, 128]) for i in range(2)]
                sg = [sb(c2, "sg%d" % d, [128, 16, 128], BF16) for d in range(2)]
                MT = [sb(c2, "MT%d" % d, [128, 16, 128], BF16) for d in range(2)]
                xdt = [sb(c2, "xdt%d" % d, [128, 1024], BF16) for d in range(2)]
                hin = [sb(c2, "hin%d" % d, [128, 1024], BF16) for d in range(2)]
                acs2 = sb(c2, "acs2", [128, 32])
                eac = sb(c2, "eac", [128, 32])
                GT = sb(c2, "GT", [128, 2, 128], BF16)
                yac = sb(c2, "yac", [128, 1024])
                ytm = sb(c2, "ytm", [128, 1024])
                szt = sb(c2, "szt", [128, 1024])
                ss2 = sb(c2, "ss2", [128, 2])
                ybf = sb(c2, "ybf", [128, 1024], BF16)
                yT = sb(c2, "yT", [128, 8, 128], BF16)
                pacs = [ps(c2, "pacs%d" % i) for i in range(4)]
                pmisc = ps(c2, "pmisc")
                py = [ps(c2, "py%d" % i) for i in range(2)]
                ptb = ps(c2, "ptb", dt=BF16)
                S.dma("sp", wn[:], ssmw_bc[:], reads=[ssmw_bc], writes=[wn])
                for c in range(16):
                    S.dma("sp", szt[:], sz_d.t[b, c * 128:(c + 1) * 128, :], reads=[sz_d], writes=[szt])
                    def mm0(e, c=c):
                        e.matmul(pmisc[:, 0:16], lhsT=tri[:, 0, :], rhs=a_t[:, c, 0:16], start=True, stop=True)
                        e.matmul(pmisc[:, 16:32], lhsT=tri[:, 1, :], rhs=a_t[:, c, 16:32], start=True, stop=True)
                        ins = None
                        for g in range(2):
                            ins = e.matmul(pmisc[:, 128 + g * 128:256 + g * 128], lhsT=BT[:, g, c * 128:(c + 1) * 128],
                                           rhs=CT[:, g, c * 128:(c + 1) * 128], start=True, stop=True)
                        return ins
                    S.op("pe", mm0, reads=[tri, a_t, BT, CT], writes=[pmisc])
                    S.op("act", lambda e: e.activation(out=acs2[:], in_=pmisc[:, 0:32], func=AF.Copy, scale=-1.0), reads=[pmisc], writes=[acs2])
                    S.op("act", lambda e: e.activation(out=eac[:], in_=pmisc[:, 0:32], func=AF.Exp), reads=[pmisc], writes=[eac])
                    S.op("act", lambda e: e.activation(out=GT[:], in_=pmisc[:, 128:384].rearrange("p (g l) -> p g l", l=128),
                                                       func=AF.Copy), reads=[pmisc], writes=[GT])
                    for d in range(2):
                        R_ = R_s[d]
                        S.dma("sp", hin[d][:], hT_d.t[b, d, c], reads=[hT_d], writes=[hin[d]])
                        S.op("pool", lambda e, d=d, c=c: e.tensor_tensor(
                            out=xdt[d][:].rearrange("p (h q) -> p h q", q=64), in0=xs_t[:, c, :].rearrange("p (h q) -> p h q", q=64),
                            in1=dt_t[:, c, d * 16:(d + 1) * 16].unsqueeze(2).to_broadcast([128, 16, 64]), op=ALU.mult),
                            reads=[xs_t, dt_t], writes=[xdt[d]], cost=2.2)
                        S.op("pool", lambda e, d=d, c=c: e.tensor_tensor(
                            out=R_[:], in0=tri[:, d, :].unsqueeze(1).to_broadcast([128, 16, 128]),
                            in1=a_t[:, c, d * 16:(d + 1) * 16].unsqueeze(2).to_broadcast([128, 16, 128]), op=ALU.mult),
                            reads=[tri, a_t], writes=[R_])
                        for q4 in range(4):
                            pa_ = pacs[q4]

                            def mma(e, pa_=pa_, q4=q4, d=d):
                                e.matmul(pa_[:], lhsT=ones_f[:], rhs=R_[:, q4 * 4:(q4 + 1) * 4, :].rearrange("p h l -> p (h l)"),
                                         start=True, stop=False)
                                return e.matmul(pa_[:], lhsT=idf_s[:], rhs=mrep[:, d, :], start=False, stop=True)
                            S.op("pe", mma, reads=[R_, ones_f, idf_s, mrep], writes=[pa_])
                            def expsub(e, pa_=pa_, q4=q4, d=d):
                                ins = None
                                for hh in range(4):
                                    h = q4 * 4 + hh
                                    ins = e.activation(out=sg[d][:, h, :], in_=pa_[:, hh * 128:(hh + 1) * 128], func=AF.Exp,
                                                       bias=acs2[:, d * 16 + h:d * 16 + h + 1])
                                return ins
                            S.op("act", expsub, reads=[pa_, acs2], writes=[sg[d]], cost=1.3)
                        for g in range(2):
                            S.op("dve", lambda e, d=d, g=g: e.tensor_tensor(
                                out=MT[d][:, g * 8:(g + 1) * 8, :], in0=sg[d][:, g * 8:(g + 1) * 8, :],
                                in1=GT[:, g, :].unsqueeze(1).to_broadcast([128, 8, 128]), op=ALU.mult),
                                reads=[sg[d], GT], writes=[MT[d]])

                    def mmy(e):
                        ins = None
                        for h in range(16):
                            dst = py[h // 8][:, (h % 8) * 64:(h % 8 + 1) * 64]
                            e.matmul(dst, lhsT=MT[0][:, h, :], rhs=xdt[0][:, h * 64:(h + 1) * 64], start=True, stop=False)
                            ins = e.matmul(dst, lhsT=MT[1][:, h, :], rhs=xdt[1][:, h * 64:(h + 1) * 64], start=False, stop=True)
                        return ins
                    S.op("pe", mmy, reads=[MT[0], MT[1], xdt[0], xdt[1]], writes=[py[0], py[1]])
                    S.op("dve", lambda e, c=c: e.tensor_tensor(out=yac[:], in0=xs_t[:, c, :], in1=dsk[:], op=ALU.mult),
                         reads=[xs_t, dsk], writes=[yac])
                    for g in range(2):
                        S.op("dve", lambda e, g=g: e.tensor_tensor(out=yac[:, g * 512:(g + 1) * 512], in0=py[g][:],
                                                                    in1=yac[:, g * 512:(g + 1) * 512], op=ALU.add),
                             reads=[py[g]], writes=[yac])
                    for d in range(2):
                        for g in range(2):
                            pyo = pacs[d * 2 + g]
                            S.op("pe", lambda e, d=d, g=g, c=c, pyo=pyo: e.matmul(
                                pyo[:], lhsT=CT[:, g, c * 128:(c + 1) * 128], rhs=hin[d][:, g * 512:(g + 1) * 512],
                                start=True, stop=True), reads=[CT, hin[d]], writes=[pyo])
                            S.op("dve", lambda e, d=d, g=g, pyo=pyo: e.tensor_tensor(
                                out=ytm[:, g * 512:(g + 1) * 512].rearrange("p (h q) -> p h q", q=64),
                                in0=pyo[:].rearrange("p (h q) -> p h q", q=64),
                                in1=eac[:, d * 16 + g * 8:d * 16 + g * 8 + 8].unsqueeze(2).to_broadcast([128, 8, 64]),
                                op=ALU.mult), reads=[pyo, eac], writes=[ytm])
                            S.op("dve", lambda e, g=g: e.tensor_tensor(
                                out=yac[:, g * 512:(g + 1) * 512], in0=yac[:, g * 512:(g + 1) * 512],
                                in1=ytm[:, g * 512:(g + 1) * 512], op=ALU.add), reads=[ytm], writes=[yac])
                    S.op("dve", lambda e: e.tensor_tensor(out=yac[:], in0=yac[:], in1=szt[:], op=ALU.mult),
                         reads=[szt], writes=[yac])
                    for g in range(2):
                        S.op("act", lambda e, g=g: e.activation(out=ytm[:, g * 512:(g + 1) * 512], in_=yac[:, g * 512:(g + 1) * 512],
                                                                func=AF.Square, accum_out=ss2[:, g:g + 1]),
                             reads=[yac], writes=[ytm, ss2])
                    S.op("dve", lambda e: e.tensor_scalar(out=ss2[:], in0=ss2[:], scalar1=1.0 / 512, scalar2=EPS,
                                                          op0=ALU.mult, op1=ALU.add), reads=[], writes=[ss2])
                    S.op("act", lambda e: e.activation(out=ss2[:], in_=ss2[:], func=AF.Sqrt), reads=[], writes=[ss2])
                    S.op("dve", lambda e: e.reciprocal(out=ss2[:], in_=ss2[:]), reads=[], writes=[ss2])
                    for g in range(2):
                        S.op("dve", lambda e, g=g: e.scalar_tensor_tensor(
                            out=ybf[:, g * 512:(g + 1) * 512], in0=yac[:, g * 512:(g + 1) * 512], scalar=ss2[:, g:g + 1],
                            in1=wn[:, g * 512:(g + 1) * 512], op0=ALU.mult, op1=ALU.mult),
                            reads=[yac, ss2, wn], writes=[ybf])

                    def tpy(e):
                        ins = None
                        for i in range(8):
                            ins = e.transpose(ptb[:, i * 128:(i + 1) * 128], ybf[:, i * 128:(i + 1) * 128], id_bf[:])
                        return ins
                    S.op("pe", tpy, reads=[ybf, id_bf], writes=[ptb])
                    S.op("act", lambda e: e.activation(out=yT[:], in_=ptb[:, 0:1024].rearrange("p (i c) -> p i c", c=128),
                                                       func=AF.Copy), reads=[ptb], writes=[yT])
                    S.dma("sp", mixT_d.t[b, 8:16, :, c * 128:(c + 1) * 128].rearrange("i p t -> p i t"), yT[:],
                          reads=[yT], writes=[mixT_d])
                S.barrier()

    def stage_E(b):
        with ExitStack() as cx:
            lg = sb(cx, "lg", [NE, T])
            with ExitStack() as c1:
                mixTs = [sb(c1, "mixT%d" % i, [128, KC, 512], BF16) for i in range(2)]
                wo = [sb(c1, "wo%d" % i, [128, KC, 512], BF16) for i in range(3)]
                h1s = [sb(c1, "h1_%d" % i, [128, KC, 512]) for i in range(2)]
                u2bs = [sb(c1, "u2b%d" % i, [128, KC, 512], BF16) for i in range(2)]
                xt = [sb(c1, "xt%d" % i, [128, 512]) for i in range(2)]
                u2f = [sb(c1, "u2f%d" % i, [128, 512]) for i in range(2)]
                sqb = [sb(c1, "sqb%d" % i, [128, 512], BF16) for i in range(2)]
                rs_ = sb(c1, "rs_", [128, 512])
                wr = sb(c1, "wr", [128, KC, NE])
                trs = [sb(c1, "trs%d" % i, [128, 1024], BF16) for i in range(2)]
                pmm = [ps(c1, "pmm%d" % i) for i in range(3)]
                pss = ps(c1, "pssE")
                plg = ps(c1, "plg")
                ptr = [ps(c1, "ptr%d" % i, dt=BF16) for i in range(2)]
                S.dma("sp", wr[:], w_rT[:], reads=[w_rT], writes=[wr])
                wov = w_out.t.rearrange("(kc p) n -> p kc n", p=128)
                n = {"w": 0, "pm": 0, "x": 0, "tr": 0}
                for tt in range(4):
                    t0 = tt * 512
                    mixT, h1, u2b = mixTs[tt % 2], h1s[tt % 2], u2bs[tt % 2]
                    S.dma("act", mixT[:], mixT_d.t[b, :, :, t0:t0 + 512].rearrange("c p t -> p c t"), reads=[mixT_d], writes=[mixT])
                    for gq in range(4):
                        w = wo[n["w"] % 3]
                        n["w"] += 1
                        S.dma("pool", w[:], wov[:, :, gq * 512:(gq + 1) * 512], reads=[w_out], writes=[w])
                        for jj in range(4):
                            j = gq * 4 + jj
                            p_ = pmm[n["pm"] % 3]
                            n["pm"] += 1
                            x_ = xt[n["x"] % 2]
                            sq_ = sqb[n["x"] % 2]
                            n["x"] += 1
                            S.dma("sp", x_[:], xT.t[b, j * 128:(j + 1) * 128, t0:t0 + 512], reads=[xT], writes=[x_])

                            def mmo(e, w=w, p_=p_, jj=jj, t0=t0):
                                ins = None
                                for kc in range(KC):
                                    ins = e.matmul(p_[:], lhsT=w[:, kc, jj * 128:(jj + 1) * 128], rhs=mixT[:, kc, :],
                                                   start=(kc == 0), stop=(kc == KC - 1))
                                return ins
                            S.op("pe", mmo, reads=[w, mixT], writes=[p_])
                            S.op("dve", lambda e, p_=p_, x_=x_, j=j: e.scalar_tensor_tensor(
                                out=h1[:, j, :], in0=p_[:], scalar=modT[:, G1 + j, b:b + 1], in1=x_[:],
                                op0=ALU.mult, op1=ALU.add), reads=[p_, x_, modT], writes=[h1])
                            S.dma("sp", h1T_d.t[b, j, :, t0:t0 + 512], h1[:, j, :], reads=[h1], writes=[h1T_d])
                            S.op("act", lambda e, sq_=sq_, j=j: e.activation(out=sq_[:], in_=h1[:, j, :], func=AF.Square),
                                 reads=[h1], writes=[sq_])
                            S.op("pe", lambda e, sq_=sq_, j=j: e.matmul(pss[:], lhsT=ones_bf[:], rhs=sq_[:],
                                                                         start=(j == 0), stop=(j == KC - 1)),
                                 reads=[sq_, ones_bf], writes=[pss])
                    S.op("dve", lambda e: e.tensor_scalar(out=rs_[:], in0=pss[:], scalar1=1.0 / D, scalar2=EPS,
                                                          op0=ALU.mult, op1=ALU.add), reads=[pss], writes=[rs_])
                    S.op("act", lambda e: e.activation(out=rs_[:], in_=rs_[:], func=AF.Sqrt), reads=[], writes=[rs_])
                    S.op("dve", lambda e: e.reciprocal(out=rs_[:], in_=rs_[:]), reads=[], writes=[rs_])
                    for j in range(KC):
                        uf = u2f[j % 2]
                        S.op("dve", lambda e, uf=uf, j=j: e.scalar_tensor_tensor(
                            out=uf[:], in0=h1[:, j, :], scalar=A2[:, j, b:b + 1], in1=rs_[:], op0=ALU.mult, op1=ALU.mult),
                            reads=[h1, rs_, A2], writes=[uf])
                        S.op("act", lambda e, uf=uf, j=j: e.activation(out=uf[:], in_=uf[:], func=AF.Identity,
                                                                        bias=modT[:, SH2 + j, b:b + 1]),
                             reads=[modT], writes=[uf])
                        S.op("pe", lambda e, uf=uf, j=j: e.matmul(plg[0:NE, :], lhsT=wr[:, j, :], rhs=uf[:],
                                                                   start=(j == 0), stop=(j == KC - 1)),
                             reads=[uf, wr], writes=[plg])
                        S.op("act", lambda e, uf=uf, j=j: e.activation(out=u2b[:, j, :], in_=uf[:], func=AF.Copy),
                             reads=[uf], writes=[u2b])
                    S.op("act", lambda e, t0=t0: e.activation(out=lg[:, t0:t0 + 512], in_=plg[0:NE, :], func=AF.Exp),
                         reads=[plg], writes=[lg])
                    for tc_ in range(4):
                        for hf in range(2):
                            pt_ = ptr[n["tr"] % 2]
                            ts_ = trs[n["tr"] % 2]
                            n["tr"] += 1

                            def tpu(e, pt_=pt_, tc_=tc_, hf=hf):
                                ins = None
                                for i in range(8):
                                    ins = e.transpose(pt_[:, i * 128:(i + 1) * 128],
                                                      u2b[:, hf * 8 + i, tc_ * 128:(tc_ + 1) * 128], id_bf[:])
                                return ins
                            S.op("pe", tpu, reads=[u2b, id_bf], writes=[pt_])
                            S.op("act", lambda e, pt_=pt_, ts_=ts_: e.activation(out=ts_[:], in_=pt_[:], func=AF.Copy),
                                 reads=[pt_], writes=[ts_])
                            r0 = t0 + tc_ * 128
                            S.dma("sp", u2tok_ds[b].t[r0:r0 + 128, hf * 1024:(hf + 1) * 1024], ts_[:], reads=[ts_], writes=[u2tok_ds[b]])
                S.barrier()
            with ExitStack() as c2:
                aff = sb(c2, "aff", [NE, T])
                W_ = sb(c2, "W_", [NE, T])
                m8 = sb(c2, "m8", [NE, 8])
                tau = sb(c2, "tau", [NE, 1])
                msk = sb(c2, "msk", [NE, T])
                gt = sb(c2, "gt", [NE, T])
                sc0 = sb(c2, "sc0", [NE, T])
                sc1 = sb(c2, "sc1", [NE, T])
                pbf = sb(c2, "pbf", [NE, T], BF16)
                pT = sb(c2, "pT", [128, 256])
                psm = ps(c2, "psm")
                ptp = ps(c2, "ptp", dt=BF16)
                for tt in range(4):
                    sl = slice(tt * 512, (tt + 1) * 512)
                    S.op("pe", lambda e, sl=sl: e.matmul(psm[0:NE, :], lhsT=ones_f[0:NE, 0:NE], rhs=lg[:, sl], start=True, stop=True),
                         reads=[lg, ones_f], writes=[psm])
                    S.op("dve", lambda e, sl=sl: e.reciprocal(out=aff[:, sl], in_=psm[0:NE, :]), reads=[psm], writes=[aff])
                    S.op("dve", lambda e, sl=sl: e.tensor_tensor(out=aff[:, sl], in0=aff[:, sl], in1=lg[:, sl], op=ALU.mult),
                         reads=[lg], writes=[aff])
                S.op("dve", lambda e: e.tensor_copy(out=W_[:], in_=aff[:]), reads=[aff], writes=[W_])
                for r_ in range(CAP // 8):
                    S.op("dve", lambda e: e.max(out=m8[:], in_=W_[:]), reads=[W_], writes=[m8])
                    if r_ < CAP // 8 - 1:
                        S.op("dve", lambda e: e.match_replace(out=W_[:], in_to_replace=m8[:], in_values=W_[:], imm_value=-1.0),
                             reads=[m8], writes=[W_])
                S.op("dve", lambda e: e.tensor_reduce(out=tau[:], in_=m8[:], axis=mybir.AxisListType.X, op=ALU.min),
                     reads=[m8], writes=[tau])
                S.op("dve", lambda e: e.tensor_scalar(out=msk[:], in0=aff[:], scalar1=tau[:, 0:1], scalar2=None, op0=ALU.is_ge),
                     reads=[aff, tau], writes=[msk])
                S.op("dve", lambda e: e.tensor_tensor(out=gt[:], in0=aff[:], in1=msk[:], op=ALU.mult), reads=[aff, msk], writes=[gt])
                S.dma("sp", gate_d.t[b], gt[:], reads=[gt], writes=[gate_d])
                if debug:
                    S.dma("sp", aff_d.t[b], aff[:], reads=[aff], writes=[aff_d])
                S.op("dve", lambda e: e.tensor_copy(out=sc0[:], in_=msk[:]), reads=[msk], writes=[sc0])
                cur, nxt = sc0, sc1
                k = 1
                while k < T:
                    S.op("dve", lambda e, cur=cur, nxt=nxt, k=k: e.tensor_copy(out=nxt[:, 0:k], in_=cur[:, 0:k]),
                         reads=[cur], writes=[nxt])
                    S.op("dve", lambda e, cur=cur, nxt=nxt, k=k: e.tensor_tensor(out=nxt[:, k:T], in0=cur[:, k:T], in1=cur[:, 0:T - k],
                                                                                 op=ALU.add), reads=[cur], writes=[nxt])
                    cur, nxt = nxt, cur
                    k *= 2
                S.op("dve", lambda e, cur=cur, nxt=nxt: e.tensor_tensor(out=nxt[:], in0=cur[:], in1=msk[:], op=ALU.mult),
                     reads=[cur, msk], writes=[nxt])
                S.op("dve", lambda e, nxt=nxt: e.tensor_scalar(out=pbf[:], in0=nxt[:], scalar1=-1.0, scalar2=None, op0=ALU.add),
                     reads=[nxt], writes=[pbf])
                S.dma("sp", pos_d.t[b], pbf[:], reads=[pbf], writes=[pos_d])

                def tpp(e):
                    ins = None
                    for tc_ in range(16):
                        ins = e.transpose(ptp[:, tc_ * 16:(tc_ + 1) * 16], pbf[:, tc_ * 128:(tc_ + 1) * 128], id_bf[0:NE, 0:NE])
                    return ins
                S.op("pe", tpp, reads=[pbf, id_bf], writes=[ptp])
                S.op("act", lambda e: e.activation(out=pT[:], in_=ptp[:, 0:256], func=AF.Copy), reads=[ptp], writes=[pT])
                S.dma("sp", posT_d.t[b], pT[:], reads=[pT], writes=[posT_d])
                S.barrier()

    def stage_F(b):
        I32 = mybir.dt.int32
        with ExitStack() as cx:
            pT = sb(cx, "pTf", [128, 16, NE])
            iota = sb(cx, "iota", [128, 256])
            tkf = sb(cx, "tkf", [128, 16, 2])
            tkb = sb(cx, "tkb", [128, 16, 2], BF16)
            sel = [sb(cx, "sel%d" % i, [128, 16, 256], BF16) for i in range(3)]
            idi = [sb(cx, "idi%d" % i, [128, 2], I32) for i in range(3)]
            Xs = [sb(cx, "Xs%d" % i, [128, D], BF16) for i in range(4)]
            XT = [sb(cx, "XTf%d" % i, [128, KC, 256], BF16) for i in range(2)]
            pix = [ps(cx, "pix%d" % i) for i in range(2)]
            ptx = [ps(cx, "ptx%d" % i, dt=BF16) for i in range(4)]
            S.dma("sp", pT[:], posT_d.t[b].rearrange("p (c e) -> p c e", e=NE), reads=[posT_d], writes=[pT])
            S.dma("sp", iota[:], iota_in[:], reads=[iota_in], writes=[iota])
            S.dma("sp", tkf[:], tokid_in[:], reads=[tokid_in], writes=[tkf])
            S.op("dve", lambda e: e.tensor_copy(out=tkb[:], in_=tkf[:]), reads=[tkf], writes=[tkb])
            nx = 0
            npt = 0
            for ex in range(NE):
                sl_, id_, px = sel[ex % 3], idi[ex % 3], pix[ex % 2]
                xt_ = XT[ex % 2]

                def mksel(e, sl_=sl_, ex=ex):
                    ins = None
                    for tc_ in range(16):
                        ins = e.tensor_scalar(out=sl_[:, tc_, :], in0=iota[:], scalar1=pT[:, tc_, ex:ex + 1], scalar2=None,
                                              op0=ALU.is_equal)
                    return ins
                S.op("dve", mksel, reads=[iota, pT], writes=[sl_], cost=5.0)

                def mmi(e, sl_=sl_, px=px):
                    ins = None
                    for h in range(2):
                        for tc_ in range(16):
                            ins = e.matmul(px[:, h * 2:h * 2 + 2], lhsT=sl_[:, tc_, h * 128:(h + 1) * 128], rhs=tkb[:, tc_, :],
                                           start=(tc_ == 0), stop=(tc_ == 15))
                    return ins
                S.op("pe", mmi, reads=[sl_, tkb], writes=[px], cost=2.5)
                S.op("dve", lambda e, px=px, id_=id_: e.tensor_scalar(
                    out=id_[:].rearrange("p (h o) -> p h o", o=1), in0=px[:, 0:4].rearrange("p (h t) -> p h t", t=2)[:, :, 0:1],
                    scalar1=128.0, scalar2=None, op0=ALU.mult), reads=[px], writes=[id_], cost=0.2)
                S.op("dve", lambda e, px=px, id_=id_: e.tensor_tensor(
                    out=id_[:].rearrange("p (h o) -> p h o", o=1), in0=id_[:].rearrange("p (h o) -> p h o", o=1),
                    in1=px[:, 0:4].rearrange("p (h t) -> p h t", t=2)[:, :, 1:2], op=ALU.add), reads=[px], writes=[id_], cost=0.2)
                for h in range(2):
                    x_ = Xs[nx % 4]
                    nx += 1
                    S.idma("pool", lambda e, x_=x_, id_=id_, h=h: e.indirect_dma_start(
                        out=x_[:, :], out_offset=None, in_=u2tok_ds[b].t[:, :],
                        in_offset=bass.IndirectOffsetOnAxis(ap=id_[:, h:h + 1], axis=0)),
                        reads=[id_, u2tok_ds[b]], writes=[x_], nbytes=128 * D * 2)
                    for k8 in range(2):
                        pt_ = ptx[npt % 4]
                        npt += 1

                        def tpx(e, pt_=pt_, x_=x_, k8=k8):
                            ins = None
                            for i in range(8):
                                kc = k8 * 8 + i
                                ins = e.transpose(pt_[:, i * 128:(i + 1) * 128], x_[:, kc * 128:(kc + 1) * 128], id_bf[:])
                            return ins
                        S.op("pe", tpx, reads=[x_, id_bf], writes=[pt_], cost=1.5)
                        S.op("act", lambda e, pt_=pt_, xt_=xt_, k8=k8, h=h: e.activation(
                            out=xt_[:, k8 * 8:(k8 + 1) * 8, h * 128:(h + 1) * 128],
                            in_=pt_[:, 0:1024].rearrange("p (i s) -> p i s", s=128), func=AF.Copy),
                            reads=[pt_], writes=[xt_], cost=0.9)
                S.dma("sp", XselT_d.t[ex, :, :, b * CAP:(b + 1) * CAP].rearrange("c p t -> p c t"), xt_[:],
                      reads=[xt_], writes=[XselT_d], nbytes=128 * KC * 256 * 2)
            S.barrier()

    def stage_G():
        NT = nb * CAP
        I32 = mybir.dt.int32
        with ExitStack() as cx:
            XT = [sb(cx, "XT%d" % i, [128, KC, NT], BF16) for i in range(2)]
            wg = [sb(cx, "wg%d" % i, [128, KC, 256], BF16) for i in range(3)]
            wu = [sb(cx, "wu%d" % i, [128, KC, 256], BF16) for i in range(3)]
            wd = [sb(cx, "wd%d" % i, [128, FC, 512], BF16) for i in range(2)]
            hid = sb(cx, "hid", [128, FC, NT], BF16)
            sg_ = [sb(cx, "sgG%d" % i, [128, NT]) for i in range(2)]
            yo = [sb(cx, "yo%d" % i, [128, 512], BF16) for i in range(3)]
            pgs = [ps(cx, "pgs%d" % i) for i in range(2)]
            pus = [ps(cx, "pus%d" % i) for i in range(2)]
            pds = [ps(cx, "pds%d" % i) for i in range(2)]
            pTs = [sb(cx, "pTg%d" % i, [128, 16, NE]) for i in range(nb)]
            iota = sb(cx, "iotaG", [128, 256])
            tkf = sb(cx, "tkf", [128, 16, 2])
            tkb = sb(cx, "tkb", [128, 16, 2], BF16)
            sel = [sb(cx, "sel%d" % i, [128, 16, 256], BF16) for i in range(2)]
            idi = [sb(cx, "idi%d" % i, [128, 2], I32) for i in range(3)]
            Xs = [sb(cx, "Xs%d" % i, [128, D], BF16) for i in range(3)]
            pix = ps(cx, "pix")
            ptx = ps(cx, "ptx", dt=BF16)
            for bb in range(nb):
                S.dma("sp", pTs[bb][:], posT_d.t[bb].rearrange("p (c e) -> p c e", e=NE), reads=[posT_d], writes=[pTs[bb]])
            S.dma("sp", iota[:], iota_in[:], reads=[iota_in], writes=[iota])
            S.dma("sp", tkf[:], tokid_in[:], reads=[tokid_in], writes=[tkf])
            S.op("dve", lambda e: e.tensor_copy(out=tkb[:], in_=tkf[:]), reads=[tkf], writes=[tkb])
            n = {"w": 0, "p": 0, "d": 0, "y": 0, "s": 0, "x": 0}

            def gather(ex):
                xt_ = XT[ex % 2]
                for bb in range(nb):
                    sl_, id_ = sel[n["s"] % 2], idi[n["s"] % 3]
                    n["s"] += 1

                    def mksel(e, sl_=sl_, ex=ex, bb=bb):
                        ins = None
                        for tc_ in range(16):
                            ins = e.tensor_scalar(out=sl_[:, tc_, :], in0=iota[:], scalar1=pTs[bb][:, tc_, ex:ex + 1], scalar2=None,
                                                  op0=ALU.is_equal)
                        return ins
                    S.op("dve", mksel, reads=[iota, pTs[bb]], writes=[sl_], cost=5.0)

                    def mmi(e, sl_=sl_):
                        ins = None
                        for h in range(2):
                            for tc_ in range(16):
                                ins = e.matmul(pix[:, h * 2:h * 2 + 2], lhsT=sl_[:, tc_, h * 128:(h + 1) * 128], rhs=tkb[:, tc_, :],
                                               start=(tc_ == 0), stop=(tc_ == 15))
                        return ins
                    S.op("pe", mmi, reads=[sl_, tkb], writes=[pix], cost=2.5)
                    S.op("dve", lambda e, id_=id_: e.tensor_scalar(
                        out=id_[:].rearrange("p (h o) -> p h o", o=1), in0=pix[:, 0:4].rearrange("p (h t) -> p h t", t=2)[:, :, 0:1],
                        scalar1=128.0, scalar2=None, op0=ALU.mult), reads=[pix], writes=[id_], cost=0.2)
                    S.op("dve", lambda e, id_=id_: e.tensor_tensor(
                        out=id_[:].rearrange("p (h o) -> p h o", o=1), in0=id_[:].rearrange("p (h o) -> p h o", o=1),
                        in1=pix[:, 0:4].rearrange("p (h t) -> p h t", t=2)[:, :, 1:2], op=ALU.add), reads=[pix], writes=[id_], cost=0.2)
                    for h in range(2):
                        x_ = Xs[n["x"] % 3]
                        n["x"] += 1
                        S.idma("pool", lambda e, x_=x_, id_=id_, h=h, bb=bb: e.indirect_dma_start(
                            out=x_[:, :], out_offset=None, in_=u2tok_ds[bb].t[:, :],
                            in_offset=bass.IndirectOffsetOnAxis(ap=id_[:, h:h + 1], axis=0)),
                            reads=[id_, u2tok_ds[bb]], writes=[x_], nbytes=128 * D * 2)
                        for k8 in range(2):
                            def tpx(e, x_=x_, k8=k8):
                                ins = None
                                for i in range(8):
                                    kc = k8 * 8 + i
                                    ins = e.transpose(ptx[:, i * 128:(i + 1) * 128], x_[:, kc * 128:(kc + 1) * 128], id_bf[:])
                                return ins
                            S.op("pe", tpx, reads=[x_, id_bf], writes=[ptx], cost=1.5)
                            c0 = bb * CAP + h * 128
                            S.op("act", lambda e, xt_=xt_, k8=k8, c0=c0: e.activation(
                                out=xt_[:, k8 * 8:(k8 + 1) * 8, c0:c0 + 128],
                                in_=ptx[:, 0:1024].rearrange("p (i s) -> p i s", s=128), func=AF.Copy),
                                reads=[ptx], writes=[xt_], cost=0.9)
                if debug:
                    S.dma("sp", XselT_d.t[ex].rearrange("c p t -> p c t"), xt_[:], reads=[xt_], writes=[XselT_d])

            gather(0)
            for ex in range(NE):
                X_ = XT[ex % 2]
                gv = w_gate.t[ex].rearrange("(kc p) f -> p kc f", p=128)
                uv = w_up.t[ex].rearrange("(kc p) f -> p kc f", p=128)
                dv = w_down.t[ex].rearrange("(fc p) d -> p fc d", p=128)
                for fg in range(11):
                    ncol = 256
                    g_, u_ = wg[n["w"] % 3], wu[n["w"] % 3]
                    n["w"] += 1
                    S.dma("pool", g_[:, :, 0:ncol], gv[:, :, fg * 256:fg * 256 + ncol], reads=[w_gate], writes=[g_], nbytes=128 * KC * 256 * 4)
                    S.dma("pool", u_[:, :, 0:ncol], uv[:, :, fg * 256:fg * 256 + ncol], reads=[w_up], writes=[u_], nbytes=128 * KC * 256 * 4)
                    if fg == 4 and ex + 1 < NE:
                        gather(ex + 1)
                    for fj in range(ncol // 128):
                        f = fg * 2 + fj
                        pg, pu = pgs[n["p"] % 2], pus[n["p"] % 2]
                        s_ = sg_[n["p"] % 2]
                        n["p"] += 1

                        def mmgu(e, g_=g_, u_=u_, pg=pg, pu=pu, fj=fj, X_=X_):
                            ins = None
                            for kc in range(KC):
                                e.matmul(pg[:, 0:NT], lhsT=g_[:, kc, fj * 128:(fj + 1) * 128], rhs=X_[:, kc, :],
                                         start=(kc == 0), stop=(kc == KC - 1))
                            for kc in range(KC):
                                ins = e.matmul(pu[:, 0:NT], lhsT=u_[:, kc, fj * 128:(fj + 1) * 128], rhs=X_[:, kc, :],
                                               start=(kc == 0), stop=(kc == KC - 1))
                            return ins
                        S.op("pe", mmgu, reads=[g_, u_, X_], writes=[pg, pu], cost=32 * NT / 2200.0)
                        S.op("act", lambda e, pg=pg, s_=s_: e.activation(out=s_[:], in_=pg[:, 0:NT], func=AF.Silu),
                             reads=[pg], writes=[s_], cost=0.6)
                        S.op("dve", lambda e, pu=pu, s_=s_, f=f: e.tensor_tensor(out=hid[:, f, :], in0=pu[:, 0:NT], in1=s_[:],
                                                                                  op=ALU.mult), reads=[pu, s_], writes=[hid], cost=0.7)
                for dg in range(4):
                    d_ = wd[n["d"] % 2]
                    n["d"] += 1
                    S.dma("pool", d_[:], dv[:, :, dg * 512:(dg + 1) * 512], reads=[w_down], writes=[d_], nbytes=128 * FC * 512 * 4)
                    for sc in range(NT // 128):
                        p_ = pds[n["y"] % 2]
                        o_ = yo[n["y"] % 3]
                        n["y"] += 1

                        def mmd(e, p_=p_, d_=d_, sc=sc):
                            ins = None
                            for f in range(FC):
                                ins = e.matmul(p_[:], lhsT=hid[:, f, sc * 128:(sc + 1) * 128], rhs=d_[:, f, :],
                                               start=(f == 0), stop=(f == FC - 1))
                            return ins
                        S.op("pe", mmd, reads=[hid, d_], writes=[p_], cost=FC * 512 / 2200.0)
                        S.op("act", lambda e, p_=p_, o_=o_: e.activation(out=o_[:], in_=p_[:], func=AF.Copy),
                             reads=[p_], writes=[o_], cost=0.6)
                        bb, hh = sc // 2, sc % 2
                        S.dma("sp", Y_d.t[bb, ex, hh * 128:(hh + 1) * 128, dg * 512:(dg + 1) * 512], o_[:],
                              reads=[o_], writes=[Y_d], nbytes=128 * 512 * 2)
            S.barrier()

    def stage_H(b):
        with ExitStack() as cx:
            posb = sb(cx, "posb", [NE, T], BF16)
            gts = sb(cx, "gts", [NE, T])
            selr_f = sb(cx, "selr_f", [NE, NE, 128])
            selr_b = sb(cx, "selr_b", [NE, NE, 128], BF16)
            jcol = sb(cx, "jcol", [128, 2])
            SGs = [sb(cx, "SG%d" % i, [128, 32, 512], BF16) for i in range(2)]
            gbc = [sb(cx, "gbc%d" % i, [128, 512]) for i in range(2)]
            Yj = [sb(cx, "Yj%d" % i, [128, 32, 256], BF16) for i in range(3)]
            h1t = [sb(cx, "h1t%d" % i, [128, 512]) for i in range(3)]
            h2 = sb(cx, "h2", [128, KC, 512])
            sqh = [sb(cx, "sqh%d" % i, [128, 512], BF16) for i in range(3)]
            rsh = sb(cx, "rsh", [128, 512])
            oo = [sb(cx, "oo%d" % i, [128, 512]) for i in range(3)]
            ppos = [ps(cx, "ppos%d" % i) for i in range(2)]
            pgat = [ps(cx, "pgat%d" % i) for i in range(2)]
            pacc = [ps(cx, "pacc%d" % i) for i in range(2)]
            pssH = ps(cx, "pssH")
            S.dma("sp", posb[:], pos_d.t[b], reads=[pos_d], writes=[posb])
            S.dma("sp", gts[:], gate_d.t[b], reads=[gate_d], writes=[gts])
            S.dma("sp", selr_f[:], selrow_in[:], reads=[selrow_in], writes=[selr_f])
            S.dma("sp", jcol[:], jcol_in[:], reads=[jcol_in], writes=[jcol])
            S.op("dve", lambda e: e.tensor_copy(out=selr_b[:], in_=selr_f[:]), reads=[selr_f], writes=[selr_b])
            n = {"e": 0, "j": 0}
            for tt in range(4):
                t0 = tt * 512
                SG = SGs[tt % 2]
                for ex in range(NE):
                    pp, pg, gb = ppos[n["e"] % 2], pgat[n["e"] % 2], gbc[n["e"] % 2]
                    n["e"] += 1
                    S.op("pe", lambda e, pp=pp, ex=ex, t0=t0: e.matmul(pp[:], lhsT=selr_b[:, ex, :], rhs=posb[:, t0:t0 + 512],
                                                                       start=True, stop=True),
                         reads=[selr_b, posb], writes=[pp])
                    S.op("pe", lambda e, pg=pg, ex=ex, t0=t0: e.matmul(pg[:], lhsT=selr_f[:, ex, :], rhs=gts[:, t0:t0 + 512],
                                                                       start=True, stop=True),
                         reads=[selr_f, gts], writes=[pg])
                    S.op("act", lambda e, pg=pg, gb=gb: e.activation(out=gb[:], in_=pg[:], func=AF.Copy), reads=[pg], writes=[gb])
                    for hh in range(2):
                        S.op("dve", lambda e, pp=pp, gb=gb, ex=ex, hh=hh: e.scalar_tensor_tensor(
                            out=SG[:, ex * 2 + hh, :], in0=pp[:], scalar=jcol[:, hh:hh + 1], in1=gb[:],
                            op0=ALU.is_equal, op1=ALU.mult), reads=[pp, gb, jcol], writes=[SG])
                for j in range(KC):
                    if j % 2 == 0:
                        y_ = Yj[(n["j"] // 2) % 3]
                        S.dma("sp" if (j // 2) % 2 else "act", y_[:],
                              Y_d.t[b, :, :, j * 128:(j + 2) * 128].rearrange("e (h p) d -> p (e h) d", p=128),
                              reads=[Y_d], writes=[y_], nbytes=128 * 32 * 256 * 2)
                    ht = h1t[n["j"] % 3]
                    sq_ = sqh[n["j"] % 3]
                    pa_ = pacc[n["j"] % 2]
                    n["j"] += 1
                    jo = (j % 2) * 128
                    S.dma("sp", ht[:], h1T_d.t[b, j, :, t0:t0 + 512], reads=[h1T_d], writes=[ht])

                    def mms(e, y_=y_, pa_=pa_, jo=jo):
                        ins = None
                        for k in range(32):
                            ins = e.matmul(pa_[:], lhsT=y_[:, k, jo:jo + 128], rhs=SG[:, k, :], start=(k == 0), stop=(k == 31))
                        return ins
                    S.op("pe", mms, reads=[y_, SG], writes=[pa_], cost=32 * 512 / 2200.0)
                    S.op("dve", lambda e, pa_=pa_, ht=ht, j=j: e.scalar_tensor_tensor(
                        out=h2[:, j, :], in0=pa_[:], scalar=modT[:, G2 + j, b:b + 1], in1=ht[:], op0=ALU.mult, op1=ALU.add),
                        reads=[pa_, ht, modT], writes=[h2])
                    S.op("act", lambda e, sq_=sq_, j=j: e.activation(out=sq_[:], in_=h2[:, j, :], func=AF.Square),
                         reads=[h2], writes=[sq_])
                    S.op("pe", lambda e, sq_=sq_, j=j: e.matmul(pssH[:], lhsT=ones_bf[:], rhs=sq_[:], start=(j == 0), stop=(j == KC - 1)),
                         reads=[sq_, ones_bf], writes=[pssH])
                S.op("dve", lambda e: e.tensor_scalar(out=rsh[:], in0=pssH[:], scalar1=1.0 / D, scalar2=EPS, op0=ALU.mult, op1=ALU.add),
                     reads=[pssH], writes=[rsh])
                S.op("act", lambda e: e.activation(out=rsh[:], in_=rsh[:], func=AF.Sqrt), reads=[], writes=[rsh])
                S.op("dve", lambda e: e.reciprocal(out=rsh[:], in_=rsh[:]), reads=[], writes=[rsh])
                for j in range(KC):
                    o_ = oo[j % 3]
                    S.op("dve", lambda e, o_=o_, j=j: e.scalar_tensor_tensor(
                        out=o_[:], in0=h2[:, j, :], scalar=fin_s[:, j:j + 1], in1=rsh[:], op0=ALU.mult, op1=ALU.mult),
                        reads=[h2, rsh, fin_s], writes=[o_])
                    S.dma("sp", outT.t[b, j * 128:(j + 1) * 128, t0:t0 + 512], o_[:], reads=[o_], writes=[outT], is_out=True)
            S.barrier()

    for b in range(nb):
        if "B" in stages:
            stage_B(b)
        if "C" in stages:
            stage_C(b)
        if "D" in stages:
            stage_D1(b)
            stage_D2(b)
        if "E" in stages:
            stage_E(b)
    if "G" in stages:
        stage_G()
    for b in range(nb):
        if "H" in stages:
            stage_H(b)

    S.finish()
    es.close()
    return nc


def rope_tables():
    quarter = 32
    freqs = (10000.0 ** (-np.arange(quarter, dtype=np.float32) / quarter)).astype(np.float32)
    pos = np.arange(T)
    row, col = (pos // 64).astype(np.float32), (pos % 64).astype(np.float32)
    cos = np.zeros((128, T), np.float32)
    sin = np.zeros((128, T), np.float32)
    for base, p in ((0, row), (64, col)):
        ang = p[None, :] * freqs[:, None]
        cos[base:base + 32] = np.cos(ang); cos[base + 32:base + 64] = np.cos(ang)
        sin[base:base + 32] = -np.sin(ang); sin[base + 32:base + 64] = np.sin(ang)
    perm = np.zeros((128, 128), np.float32)
    for m in range(128):
        partner = m + 32 if (m % 64) < 32 else m - 32
        perm[partner, m] = 1.0
    return cos, sin, perm


def _attn_consts():
    ridx = np.zeros((128, 35, 128), np.int64)
    cidx = np.zeros((128, 35, 128), np.int64)
    mask = np.zeros((128, 35, 128), np.float32)
    key = np.arange(128)[:, None]
    q = np.arange(128)[None, :]
    for case, rp in enumerate((0, 1, 7, 14, 15)):
        r0 = 2 * rp
        start = min(max(r0 - 4, 0), 22)
        qr = r0 + q // 64
        qc = q % 64
        rs = np.clip(qr - 4, 0, 24)
        cs = np.clip(qc - 8, 0, 48)
        for j in range(5):
            kr = start + 2 * j + key // 64
            kc = key % 64
            valid = (kr >= rs) & (kr < rs + 8) & (kc >= cs) & (kc < cs + 16)
            ridx[:, case * 7 + j, :] = np.clip(kr - qr + 7, 0, 14)
            cidx[:, case * 7 + j, :] = np.clip(kc - qc + 15, 0, 30)
            mask[:, case * 7 + j, :] = np.where(valid, 0.0, NEG)
    valid_all = mask == 0.0

    def gather(rpb):
        g = rpb[:, ridx, cidx]
        g = np.where(valid_all[None], g, 0.0).astype(np.float32)
        for case in range(5):
            g[:, :, case * 7 + 5:case * 7 + 7, :] = 0.0
        return np.ascontiguousarray(g)
    for case in range(5):
        mask[:, case * 7 + 5:case * 7 + 7, :] = 0.0
    s_ = np.arange(128)[:, None]
    l_ = np.arange(128)[None, :]
    tri = np.stack([(s_ <= l_), (s_ >= l_)]).astype(np.float32)
    m2 = np.stack([np.where(l_ >= s_, 0.0, NEG), np.where(l_ <= s_, 0.0, NEG)]).astype(np.float32)
    mrep = np.ascontiguousarray(np.tile(m2, (1, 1, 4)))
    return {"rpb_idx": gather, "amask": mask, "tri": tri, "mrep": mrep}


CONSTS = _attn_consts()


def fm_cols(v):
    return np.ascontiguousarray(v.reshape(-1, 128).T)


def make_in_maps(inp, nb=NB, cores=8):
    cos, sin, perm = rope_tables()
    maps = []
    for c in range(cores):
        bs = [c * nb + i for i in range(nb)]
        m = {}
        m["xT"] = np.ascontiguousarray(np.transpose(inp["x"][bs], (0, 2, 1)))
        m["ctxT"] = np.ascontiguousarray(np.transpose(inp["ctx"][bs], (0, 2, 1)))
        conds = [inp["c"][bs[0]], inp["c"][bs[-1]], inp["c_ctx"]]
        m["cT"] = np.ascontiguousarray(np.stack([fm_cols(v) for v in conds], axis=-1))
        m["w_ada"] = inp["w_ada"][0]
        m["b_adaT"] = fm_cols(inp["b_ada"][0])
        m["nmixT"] = fm_cols(inp["norm_mix_w"][0])
        m["nffnT"] = fm_cols(inp["norm_ffn_w"][0])
        m["finT"] = fm_cols(inp["final_norm_w"])
        m["w_in"] = inp["w_in"][0]
        m["cos_t"] = cos; m["sin_t"] = sin; m["permT"] = perm
        m["dtb_bc"] = np.ascontiguousarray(np.broadcast_to(inp["dt_bias"][0].reshape(1, 32), (128, 32)))
        m["ident_f"] = np.eye(128, dtype=np.float32)
        m["rpbG"] = CONSTS["rpb_idx"](inp["rpb"][0])
        m["amask"] = CONSTS["amask"]
        m["tri_in"] = CONSTS["tri"]
        m["mrep_in"] = CONSTS["mrep"]
        m["alog_bc"] = np.ascontiguousarray(np.broadcast_to(inp["a_log"][0].reshape(1, 32), (128, 32)))
        m["dskip_bc"] = np.ascontiguousarray(np.broadcast_to(np.repeat(inp["d_skip"][0], 64).reshape(1, 1024), (128, 1024)))
        m["ssmw_bc"] = np.ascontiguousarray(np.broadcast_to(inp["ssm_norm_w"][0].reshape(1, 1024), (128, 1024)))
        m["convw"] = np.ascontiguousarray(inp["conv_w"][0].reshape(5, 12, 128).transpose(2, 1, 0))
        m["convb"] = fm_cols(inp["conv_b"][0])
        if "w_out" in inp:
            m["w_out"] = inp["w_out"][0]
            m["w_rT"] = np.ascontiguousarray(inp["w_router"][0].reshape(KC, 128, NE).transpose(1, 0, 2))
            sr = np.zeros((NE, NE, 128), np.float32)
            for e_ in range(NE):
                sr[e_, e_, :] = 1.0
            m["selrow_in"] = sr
            m["iota_in"] = np.ascontiguousarray(np.broadcast_to(np.arange(256, dtype=np.float32)[None], (128, 256)))
            m["jcol_in"] = np.stack([np.arange(128), np.arange(128) + 128], axis=1).astype(np.float32)
            tk = np.zeros((128, 16, 2), np.float32)
            tk[:, :, 0] = np.arange(16)[None, :]
            tk[:, :, 1] = np.arange(128)[:, None]
            m["tokid_in"] = tk
        if "w_gate" in inp:
            m["w_gate"] = inp["w_gate"][0]
            m["w_up"] = inp["w_up"][0]
            m["w_down"] = inp["w_down"][0]
        maps.append(m)
    return maps


def kernel(**inp):
    inp = {k: np.asarray(v) for k, v in inp.items()}
    nc = build()
    maps = make_in_maps(inp)
    res = run_bass_kernel_spmd(nc, maps, core_ids=list(range(8)))
    out = np.zeros((16, T, D), np.float32)
    for c in range(8):
        o = res.results[c]["outT"]
        for i in range(NB):
            out[c * NB + i] = o[i].T
    return out
```

```python
import math, os, types
BCUT = int(os.environ.get('BCUT', '9'))
QCUT = int(os.environ.get('QCUT', '9'))
from contextlib import ExitStack
import numpy as np
import concourse.bass as bass
import concourse.mybir as mybir
from concourse.bass_utils import run_bass_kernel_spmd

F32, BF16 = mybir.dt.float32, mybir.dt.bfloat16
ALU = mybir.AluOpType
AF = mybir.ActivationFunctionType

D = 2048; T = 2048; L = 256; NB = 2; KC = 16
DIN = 5664; NH = 8; DH = 128; HS = 16; PS = 64; NG = 2; NS = 128
NE = 16; CAP = 256; FF = 2816; FC = 22
EPS = 1e-6
NEG = -30000.0


class Res:
    __slots__ = ("gen", "w", "r")

    def __init__(self):
        self.gen = -1
        self.w = []
        self.r = []


class Tl:
    def __init__(self, t, excl=False, multi=False):
        self.t = t
        self.res = Res()
        self.excl = excl
        self.multi = multi

    def __getitem__(self, k):
        return self.t[k]


def _freeze(fn):
    if fn.__closure__ is None:
        return fn
    cells = []
    for c in fn.__closure__:
        try:
            cells.append(types.CellType(c.cell_contents))
        except ValueError:
            cells.append(c)
    return types.FunctionType(fn.__code__, fn.__globals__, fn.__name__, fn.__defaults__, tuple(cells))


class Node:
    __slots__ = ("idx", "eng", "fn", "dma", "deps", "succ", "cost", "bytes", "is_out", "tok", "nd")


class Sched:
    def __init__(self, nc, es):
        self.nc = nc
        self.eng = {"pe": nc.tensor, "act": nc.scalar, "dve": nc.vector, "pool": nc.gpsimd, "sp": nc.sync}
        self.sem = {e: es.enter_context(nc.semaphore("s_" + e)) for e in self.eng}
        self.cnt = {e: 0 for e in self.eng}
        self.seen = {e: {} for e in self.eng}
        self.dsem = {}
        for q, n in (("sp", 12), ("pool", 8), ("act", 6)):
            self.dsem[q] = [[es.enter_context(nc.semaphore("d_%s%d" % (q, i))), 0] for i in range(n)]
        self.drr = {q: 0 for q in self.dsem}
        self.out_tokens = []
        self.nodes = []
        self.gen = 0

    def _res(self, t):
        r = t.res
        if r.gen != self.gen:
            r.gen, r.w, r.r = self.gen, [], []
        return r

    def _record(self, nd, reads, writes):
        writes = list(writes) + [r for r in reads if r.excl]
        reads = [r for r in reads if not r.excl]
        deps = set()
        for t in reads:
            deps.update(self._res(t).w)
        for t in writes:
            r = self._res(t)
            if not t.multi:
                deps.update(r.w)
                deps.update(r.r)
        nd.idx = len(self.nodes)
        nd.deps = deps
        nd.succ = []
        nd.tok = None
        self.nodes.append(nd)
        for t in reads:
            r = self._res(t)
            r.r.append(nd.idx)
            if len(r.r) > 64:
                r.r = r.r[-64:]
        for t in writes:
            r = self._res(t)
            if t.multi:
                r.w.append(nd.idx)
            else:
                r.w = [nd.idx]
                r.r = []

    def op(self, e, fn, reads=(), writes=(), cost=0.6):
        nd = Node()
        nd.eng, nd.fn, nd.dma, nd.cost, nd.bytes, nd.is_out = e, _freeze(fn), None, cost, 0, False
        self._record(nd, reads, writes)

    def dma(self, q, out, in_, reads=(), writes=(), is_out=False, nbytes=1 << 20, **kw):
        nd = Node()
        nd.eng, nd.fn, nd.dma, nd.cost, nd.bytes, nd.is_out = q, None, (out, in_, kw), 0.06, nbytes, is_out
        self._record(nd, reads, writes)

    def idma(self, q, fn, reads=(), writes=(), nbytes=1 << 20):
        nd = Node()
        nd.eng, nd.fn, nd.dma, nd.cost, nd.bytes, nd.is_out = q, None, (_freeze(fn),), 0.3, nbytes, False
        self._record(nd, reads, writes)

    def _wait(self, e, tok):
        sem, val = tok
        k = id(sem)
        if self.seen[e].get(k, 0) >= val:
            return
        self.seen[e][k] = val
        self.eng[e].wait_ge(sem, val)

    def flush(self):
        nodes = self.nodes
        n = len(nodes)
        if n == 0:
            return
        for nd in nodes:
            nd.nd = len(nd.deps)
            for d in nd.deps:
                nodes[d].succ.append(nd.idx)
        ready_t = [0.0] * n
        finish = [0.0] * n
        avail = {e: [] for e in self.eng}
        free = {e: 0.0 for e in self.eng}
        for nd in nodes:
            if nd.nd == 0:
                avail[nd.eng].append(nd.idx)
        dma_free = 0.0
        order = []
        left = n
        while left:
            best = None
            for e, lst in avail.items():
                if not lst:
                    continue
                fe = free[e]
                cand = None
                for i in lst:
                    st = ready_t[i] if ready_t[i] > fe else fe
                    key = (st, i)
                    if cand is None or key < cand:
                        cand = key
                if best is None or cand < best[0]:
                    best = (cand, e)
            (st, i), e = best
            avail[e].remove(i)
            nd = nodes[i]
            if nd.dma is not None:
                free[e] = st + nd.cost
                xs = max(st + 0.3, dma_free)
                dma_free = xs + nd.bytes / 250e3
                finish[i] = dma_free + 1.7
            else:
                free[e] = st + nd.cost
                finish[i] = st + nd.cost + 0.15
            order.append(i)
            left -= 1
            for sidx in nd.succ:
                sn = nodes[sidx]
                if finish[i] > ready_t[sidx]:
                    ready_t[sidx] = finish[i]
                sn.nd -= 1
                if sn.nd == 0:
                    avail[sn.eng].append(sidx)
        for i in order:
            nd = nodes[i]
            e = nd.eng
            for d in sorted(nd.deps):
                self._wait(e, nodes[d].tok)
            if nd.dma is not None:
                k = self.drr[e]
                self.drr[e] = (k + 1) % len(self.dsem[e])
                slot = self.dsem[e][k]
                if slot[1]:
                    self._wait(e, (slot[0], slot[1]))
                slot[1] += 16
                if len(nd.dma) == 1:
                    nd.dma[0](self.eng[e]).then_inc(slot[0], 16)
                else:
                    out, in_, kw = nd.dma
                    self.eng[e].dma_start(out=out, in_=in_, **kw).then_inc(slot[0], 16)
                nd.tok = (slot[0], slot[1])
                if nd.is_out:
                    self.out_tokens.append(nd.tok)
            else:
                ins = nd.fn(self.eng[e])
                self.cnt[e] += 1
                ins.then_inc(self.sem[e], 1)
                nd.tok = (self.sem[e], self.cnt[e])
            nd.fn = None
            nd.dma = None
        self.nodes = []
        self.gen += 1

    def barrier(self):
        self.flush()
        toks = [(self.sem[e], self.cnt[e]) for e in self.eng if self.cnt[e]]
        for q in self.dsem:
            toks += [(s[0], s[1]) for s in self.dsem[q] if s[1]]
        for e in self.eng:
            for t in toks:
                self._wait(e, t)

    def finish(self):
        self.flush()
        for t in self.out_tokens:
            self._wait("sp", t)
        self.barrier()


def build(nb=NB, stages="ABCDEFGH", debug=False, kinds="qkvzxd", b1=True):
    nc = bass.Bass("TRN2", target_bir_lowering=False)
    es = ExitStack()
    S = Sched(nc, es)
    okind = "ExternalOutput" if debug else "Internal"

    def din(name, shape, dt=F32):
        return Tl(nc.dram_tensor(name, list(shape), dt, kind="ExternalInput").ap())

    def dscr(name, shape, dt=F32):
        return Tl(nc.dram_tensor(name, list(shape), dt, kind=okind).ap(), multi=True)

    uid = [0]

    def sb(ctx, name, shape, dt=F32):
        uid[0] += 1
        return Tl(ctx.enter_context(nc.sbuf_tensor("%s_%d" % (name, uid[0]), list(shape), dt)))

    def ps(ctx, name, shape=None, dt=F32):
        uid[0] += 1
        return Tl(ctx.enter_context(nc.psum_tensor("%s_%d" % (name, uid[0]), [128, 512] if dt == F32 else [128, 1024], dt)), excl=True)

    xT = din("xT", [nb, D, T])
    ctxT = din("ctxT", [nb, D, L])
    cT = din("cT", [128, KC, 3])
    w_ada = din("w_ada", [D, 6 * D])
    b_adaT = din("b_adaT", [128, 96])
    nmixT = din("nmixT", [128, KC])
    nffnT = din("nffnT", [128, KC])
    finT = din("finT", [128, KC])
    w_in = din("w_in", [D, DIN])
    cos_t = din("cos_t", [128, T])
    sin_t = din("sin_t", [128, T])
    permT = din("permT", [128, 128])
    dtb_bc = din("dtb_bc", [128, 32])
    ident_f = din("ident_f", [128, 128])
    rpbG = din("rpbG", [NH, 128, 35, 128])
    amask = din("amask", [128, 35, 128])
    tri_in = din("tri_in", [2, 128, 128])
    mrep_in = din("mrep_in", [2, 128, 512])
    alog_bc = din("alog_bc", [128, 32])
    dskip_bc = din("dskip_bc", [128, 1024])
    ssmw_bc = din("ssmw_bc", [128, 1024])
    convw = din("convw", [128, 12, 5])
    convb = din("convb", [128, 12])
    w_out = din("w_out", [D, D])
    w_rT = din("w_rT", [128, KC, NE])
    selrow_in = din("selrow_in", [NE, NE, 128])
    iota_in = din("iota_in", [128, 256])
    jcol_in = din("jcol_in", [128, 2])
    tokid_in = din("tokid_in", [128, 16, 2])
    if "G" in stages:
        w_gate = din("w_gate", [NE, D, FF])
        w_up = din("w_up", [NE, D, FF])
        w_down = din("w_down", [NE, FF, D])

    qT_d = dscr("qT_d", [nb, NH, 128, T], BF16)
    kT_d = dscr("kT_d", [nb, NH, 128, T + L], BF16)
    v_d = dscr("v_d", [nb, T + L, NH * DH], BF16)
    sz_d = dscr("sz_d", [nb, T, 1024], F32)
    xbcT_d = dscr("xbcT_d", [nb, 12, 128, T + L], F32)
    dt_d = dscr("dt_d", [nb, T + L, 32], F32)
    mod_d = dscr("mod_d", [128, 96, 3], F32)
    mixT_d = dscr("mixT_d", [nb, 16, 128, T], BF16)
    xs_tok_d = dscr("xs_tok_d", [nb, T + L, 1024], BF16)
    B_tok_d = dscr("B_tok_d", [nb, T + L, 256], BF16)
    BT_d = dscr("BT_d", [nb, 2, 128, T + L], BF16)
    CT_d = dscr("CT_d", [nb, 2, 128, T + L], BF16)
    hT_d = dscr("hT_d", [nb, 2, 16, 128, 1024], BF16)
    h1T_d = dscr("h1T_d", [nb, KC, 128, T], F32)
    u2tok_ds = [dscr("u2tok_d%d" % i, [T, D], BF16) for i in range(nb)]
    pos_d = dscr("pos_d", [nb, NE, T], BF16)
    gate_d = dscr("gate_d", [nb, NE, T], F32)
    posT_d = dscr("posT_d", [nb, 128, 256], F32)
    aff_d = dscr("aff_d", [nb, NE, T], F32)
    XselT_d = dscr("XselT_d", [NE, KC, 128, nb * CAP], BF16)
    Y_d = dscr("Y_d", [nb, NE, CAP, D], BF16)
    outT = Tl(nc.dram_tensor("outT", [nb, D, T], F32, kind="ExternalOutput").ap(), multi=True)

    ones_bf = sb(es, "ones_bf", [128, 128], BF16)
    modT = sb(es, "modT", [128, 96, 3])
    A1 = sb(es, "A1", [128, KC, 3])
    A2 = sb(es, "A2", [128, KC, 3])
    nmix_s = sb(es, "nmix_s", [128, KC])
    nffn_s = sb(es, "nffn_s", [128, KC])
    fin_s = sb(es, "fin_s", [128, KC])
    S.op("dve", lambda e: e.memset(ones_bf[:], 1.0), writes=[ones_bf])
    ones_f = sb(es, "ones_f", [128, 128])
    S.op("dve", lambda e: e.memset(ones_f[:], 1.0), writes=[ones_f])
    idf_s = sb(es, "idf_s", [128, 128])
    id_bf = sb(es, "id_bf", [128, 128], BF16)
    S.dma("sp", idf_s[:], ident_f[:], reads=[ident_f], writes=[idf_s])
    S.op("dve", lambda e: e.tensor_copy(out=id_bf[:], in_=idf_s[:]), reads=[idf_s], writes=[id_bf])
    S.dma("sp", nmix_s[:], nmixT[:], reads=[nmixT], writes=[nmix_s])
    S.dma("sp", nffn_s[:], nffnT[:], reads=[nffnT], writes=[nffn_s])
    S.dma("sp", fin_s[:], finT[:], reads=[finT], writes=[fin_s])

    if "A" in stages:
        with ExitStack() as cx:
            c_s = sb(cx, "c_s", [128, KC, 3])
            sc_bf = sb(cx, "sc_bf", [128, KC, 3], BF16)
            bada_s = sb(cx, "bada_s", [128, 96])
            wa = [sb(cx, "wa%d" % i, [128, KC, 1024], BF16) for i in range(2)]
            pa = [ps(cx, "pa%d" % i) for i in range(2)]
            S.dma("sp", c_s[:], cT[:], reads=[cT], writes=[c_s])
            S.dma("sp", bada_s[:], b_adaT[:], reads=[b_adaT], writes=[bada_s])
            S.op("act", lambda e: e.activation(out=sc_bf[:], in_=c_s[:], func=AF.Silu), reads=[c_s], writes=[sc_bf])
            wv = w_ada.t.rearrange("(kc p) n -> p kc n", p=128)
            for g in range(12):
                w = wa[g % 2]
                p_ = pa[g % 2]
                S.dma("pool", w[:], wv[:, :, g * 1024:(g + 1) * 1024], reads=[w_ada], writes=[w])

                def mm(e, w=w, p_=p_):
                    ins = None
                    for j in range(8):
                        for kc in range(KC):
                            ins = e.matmul(p_[:, j * 4:j * 4 + 3], lhsT=w[:, kc, j * 128:(j + 1) * 128], rhs=sc_bf[:, kc, :],
                                           start=(kc == 0), stop=(kc == KC - 1))
                    return ins
                S.op("pe", mm, reads=[w, sc_bf], writes=[p_])
                S.op("dve", lambda e, g=g, p_=p_: e.tensor_tensor(
                    out=modT[:, g * 8:(g + 1) * 8, :], in0=p_[:, 0:32].rearrange("p (j r) -> p j r", r=4)[:, :, 0:3],
                    in1=bada_s[:, g * 8:(g + 1) * 8].unsqueeze(2).to_broadcast([128, 8, 3]), op=ALU.add),
                    reads=[p_, bada_s], writes=[modT])
            for (A_, n_, off) in ((A1, nmix_s, 16), (A2, nffn_s, 64)):
                S.op("dve", lambda e, A_=A_, n_=n_, off=off: e.scalar_tensor_tensor(
                    out=A_[:], in0=modT[:, off:off + 16, :], scalar=1.0,
                    in1=n_[:].unsqueeze(2).to_broadcast([128, KC, 3]), op0=ALU.add, op1=ALU.mult),
                    reads=[modT, n_], writes=[A_])
            if debug:
                S.dma("sp", mod_d[:], modT[:], reads=[modT], writes=[mod_d], is_out=True)
            S.barrier()

    SH1, G1, SH2, G2 = 0, 32, 48, 80

    def stage_B(b):
        with ExitStack() as cx:
            uT_lat = [sb(cx, "uT%d" % i, [128, KC, 512], BF16) for i in range(4)]
            uT_ctx = [sb(cx, "uTc", [128, KC, L], BF16)]
            xs = [sb(cx, "xs%d" % i, [128, KC, 256]) for i in range(2)]
            sq = [sb(cx, "sq%d" % i, [128, KC, 256], BF16) for i in range(2)]
            rstd = [sb(cx, "rstd%d" % i, [128, 256]) for i in range(2)]
            tmp = [sb(cx, "tmpB%d" % i, [128, 512]) for i in range(3)]
            wi = [sb(cx, "wi%d" % i, [128, KC, 512], BF16) for i in range(2)]
            cos_s = sb(cx, "cos_s", [128, T])
            sin_s = sb(cx, "sin_s", [128, T])
            perm_f = sb(cx, "perm_f", [128, 128])
            perm_s = sb(cx, "perm_s", [128, 128], BF16)
            dtb_s = sb(cx, "dtb_s", [128, 32])
            qsb = [sb(cx, "qsb%d" % i, [128, 512], BF16) for i in range(2)]
            ost = [sb(cx, "ost%d" % i, [128, 512]) for i in range(3)]
            ostb = [sb(cx, "ostb%d" % i, [128, 512], BF16) for i in range(3)]
            pss = ps(cx, "pss", [128, 256])
            pm = [ps(cx, "pm%d" % i, [128, 512]) for i in range(4)]
            pw = [ps(cx, "pw%d" % i, [128, 512]) for i in range(2)]
            S.dma("sp", cos_s[:], cos_t[:], reads=[cos_t], writes=[cos_s])
            S.dma("sp", sin_s[:], sin_t[:], reads=[sin_t], writes=[sin_s])
            S.dma("sp", perm_f[:], permT[:], reads=[permT], writes=[perm_f])
            S.dma("sp", dtb_s[:], dtb_bc[:], reads=[dtb_bc], writes=[dtb_s])
            S.op("dve", lambda e: e.tensor_copy(out=perm_s[:], in_=perm_f[:]), reads=[perm_f], writes=[perm_s])
            wv = w_in.t.rearrange("(kc p) n -> p kc n", p=128)
            cnt = {"x": 0, "pm": 0, "pw": 0, "ost": 0, "w": 0, "tmp": 0, "q": 0}

            for (src, ntok, r, tok0) in ((ctxT, L, 2, T), (xT, T, b, 0)):
                is_ctx = (r == 2)
                uT = uT_ctx if is_ctx else uT_lat
                sv = src.t[b].rearrange("(kc p) t -> p kc t", p=128)
                for t0 in (range(0, ntok, 256) if b1 else []):
                    i = cnt["x"] % 2
                    cnt["x"] += 1
                    x_, s_, r_ = xs[i], sq[i], rstd[i]
                    S.dma("sp", x_[:], sv[:, :, t0:t0 + 256], reads=[src], writes=[x_])
                    if BCUT < 1: continue
                    S.op("act", lambda e, x_=x_, s_=s_: e.activation(out=s_[:], in_=x_[:], func=AF.Square),
                         reads=[x_], writes=[s_])
                    if BCUT < 2: continue

                    def mmss(e, s_=s_):
                        ins = None
                        for kc in range(KC):
                            ins = e.matmul(pss[:, 0:256], lhsT=ones_bf[:], rhs=s_[:, kc, :], start=(kc == 0), stop=(kc == KC - 1))
                        return ins
                    S.op("pe", mmss, reads=[s_, ones_bf], writes=[pss])
                    if BCUT < 3: continue
                    S.op("dve", lambda e, r_=r_: e.tensor_scalar(out=r_[:], in0=pss[:, 0:256], scalar1=1.0 / D, scalar2=EPS,
                                                                  op0=ALU.mult, op1=ALU.add), reads=[pss], writes=[r_])
                    S.op("act", lambda e, r_=r_: e.activation(out=r_[:], in_=r_[:], func=AF.Sqrt), reads=[r_], writes=[r_])
                    S.op("dve", lambda e, r_=r_: e.reciprocal(out=r_[:], in_=r_[:]), reads=[r_], writes=[r_])
                    for kc in range(KC if BCUT >= 4 else 0):
                        tm = tmp[cnt["tmp"] % 3]
                        cnt["tmp"] += 1
                        S.op("dve", lambda e, x_=x_, r_=r_, tm=tm, kc=kc: e.scalar_tensor_tensor(
                            out=tm[:, 0:256], in0=x_[:, kc, :], scalar=A1[:, kc, r:r + 1], in1=r_[:],
                            op0=ALU.mult, op1=ALU.mult), reads=[x_, r_, A1], writes=[tm])
                        uq = uT[t0 // 512]
                        S.op("act", lambda e, tm=tm, kc=kc, t0=t0, uq=uq: e.activation(
                            out=uq[:, kc, t0 % 512:t0 % 512 + 256], in_=tm[:, 0:256], func=AF.Identity,
                            bias=modT[:, SH1 + kc, r:r + 1]), reads=[tm, modT], writes=[uq])

                tts = [(t0, min(512, ntok - t0)) for t0 in range(0, ntok, 512)]
                for g in range(12):
                    kind = ("q", "q", "k", "k", "v", "v", "z", "z", "x", "x", "x", "d")[g]
                    if (is_ctx and kind in ("q", "z")) or kind not in kinds:
                        continue
                    ncol = 512 if g < 11 else 32
                    w = wi[cnt["w"] % 2]
                    cnt["w"] += 1
                    S.dma("pool", w[:, :, 0:ncol], wv[:, :, g * 512:g * 512 + ncol], reads=[w_in], writes=[w])
                    if kind in ("v", "d", "z"):
                        for tc_ in range(ntok // 128):
                            p_ = pm[cnt["pm"] % 4]
                            cnt["pm"] += 1

                            uq = uT[tc_ // 4]

                            def mmv(e, w=w, p_=p_, tc_=tc_, ncol=ncol, uq=uq):
                                ins = None
                                for kc in range(KC):
                                    ins = e.matmul(p_[:, 0:ncol], lhsT=uq[:, kc, (tc_ % 4) * 128:(tc_ % 4 + 1) * 128],
                                                   rhs=w[:, kc, 0:ncol], start=(kc == 0), stop=(kc == KC - 1))
                                return ins
                            S.op("pe", mmv, reads=[w, uq], writes=[p_], cost=16 * max(ncol, 64) / 2200.0)
                            row0 = tok0 + tc_ * 128
                            if kind == "z":
                                o_ = ost[cnt["ost"] % 3]
                                cnt["ost"] += 1
                                S.op("act", lambda e, o_=o_, p_=p_: e.activation(out=o_[:], in_=p_[:], func=AF.Silu),
                                     reads=[p_], writes=[o_])
                                c0 = (g - 6) * 512
                                S.dma("sp", sz_d.t[b, row0:row0 + 128, c0:c0 + 512], o_[:], reads=[o_], writes=[sz_d])
                            elif kind == "v":
                                o_ = ostb[cnt["ost"] % 3]
                                cnt["ost"] += 1
                                S.op("act", lambda e, o_=o_, p_=p_: e.activation(out=o_[:], in_=p_[:], func=AF.Copy),
                                     reads=[p_], writes=[o_])
                                c0 = (g - 4) * 512
                                S.dma("sp", v_d.t[b, row0:row0 + 128, c0:c0 + 512], o_[:], reads=[o_], writes=[v_d])
                            else:
                                o_ = ost[cnt["ost"] % 3]
                                cnt["ost"] += 1
                                S.op("dve", lambda e, o_=o_, p_=p_: e.tensor_tensor(out=o_[:, 0:32], in0=p_[:, 0:32],
                                                                                   in1=dtb_s[:], op=ALU.add),
                                     reads=[p_, dtb_s], writes=[o_])
                                S.op("act", lambda e, o_=o_: e.activation(out=o_[:, 0:32], in_=o_[:, 0:32], func=AF.Exp),
                                     reads=[o_], writes=[o_])
                                S.op("act", lambda e, o_=o_: e.activation(out=o_[:, 0:32], in_=o_[:, 0:32], func=AF.Ln,
                                                                          bias=1.0), reads=[o_], writes=[o_])
                                S.dma("sp", dt_d.t[b, row0:row0 + 128, :], o_[:, 0:32], reads=[o_], writes=[dt_d])
                        continue
                    for j in range(4):
                        ch = g * 4 + j
                        for (t0, tn) in tts:
                            p_ = pm[cnt["pm"] % 4]
                            cnt["pm"] += 1

                            uq = uT[t0 // 512]

                            def mmf(e, w=w, p_=p_, j=j, t0=t0, tn=tn, uq=uq):
                                ins = None
                                for kc in range(KC):
                                    ins = e.matmul(p_[:, 0:tn], lhsT=w[:, kc, j * 128:(j + 1) * 128],
                                                   rhs=uq[:, kc, 0:tn], start=(kc == 0), stop=(kc == KC - 1))
                                return ins
                            S.op("pe", mmf, reads=[w, uq], writes=[p_], cost=16 * tn / 2200.0)
                            if kind in ("q", "k") and not is_ctx:
                                qb = qsb[cnt["q"] % 2]
                                p2 = pw[cnt["q"] % 2]
                                cnt["q"] += 1
                                S.op("act", lambda e, qb=qb, p_=p_: e.activation(out=qb[:], in_=p_[:], func=AF.Copy),
                                     reads=[p_], writes=[qb])
                                if QCUT < 2: continue
                                S.op("pe", lambda e, qb=qb, p2=p2: e.matmul(p2[:], lhsT=perm_s[:], rhs=qb[:],
                                                                              start=True, stop=True),
                                     reads=[qb, perm_s], writes=[p2])
                                if QCUT < 3: continue
                                ta = tmp[cnt["tmp"] % 3]
                                tb = tmp[(cnt["tmp"] + 1) % 3]
                                cnt["tmp"] += 2
                                S.op("dve", lambda e, ta=ta, p_=p_, t0=t0: e.tensor_tensor(
                                    out=ta[:], in0=p_[:], in1=cos_s[:, t0:t0 + 512], op=ALU.mult),
                                    reads=[p_, cos_s], writes=[ta])
                                S.op("dve", lambda e, tb=tb, p2=p2, t0=t0: e.tensor_tensor(
                                    out=tb[:], in0=p2[:], in1=sin_s[:, t0:t0 + 512], op=ALU.mult),
                                    reads=[p2, sin_s], writes=[tb])
                                if QCUT < 4: continue
                                o_ = ostb[cnt["ost"] % 3]
                                cnt["ost"] += 1
                                S.op("dve", lambda e, o_=o_, ta=ta, tb=tb: e.tensor_tensor(
                                    out=o_[:], in0=ta[:], in1=tb[:], op=ALU.add), reads=[ta, tb], writes=[o_])
                                if QCUT < 5: continue
                                dst = qT_d if kind == "q" else kT_d
                                h = ch % 8
                                S.dma("sp", dst.t[b, h, :, t0:t0 + 512], o_[:], reads=[o_], writes=[dst])
                            elif kind == "k":
                                o_ = ostb[cnt["ost"] % 3]
                                cnt["ost"] += 1
                                S.op("act", lambda e, o_=o_, p_=p_, tn=tn: e.activation(out=o_[:, 0:tn], in_=p_[:, 0:tn],
                                                                                        func=AF.Copy),
                                     reads=[p_], writes=[o_])
                                S.dma("sp", kT_d.t[b, ch % 8, :, T + t0:T + t0 + tn], o_[:, 0:tn], reads=[o_], writes=[kT_d])
                            else:
                                o_ = ost[cnt["ost"] % 3]
                                cnt["ost"] += 1
                                S.op("act", lambda e, o_=o_, p_=p_, tn=tn: e.activation(out=o_[:, 0:tn], in_=p_[:, 0:tn],
                                                                                        func=AF.Copy),
                                     reads=[p_], writes=[o_])
                                S.dma("sp", xbcT_d.t[b, ch - 32, :, tok0 + t0:tok0 + t0 + tn], o_[:, 0:tn],
                                      reads=[o_], writes=[xbcT_d])
            S.barrier()


    def stage_C(b):
        scale = DH ** -0.5
        with ExitStack() as cx:
            am_s = sb(cx, "am_s", [128, 35, 128])
            bias = [sb(cx, "bias%d" % i, [128, 35, 128]) for i in range(2)]
            biasb = [sb(cx, "biasb%d" % i, [128, 35, 128], BF16) for i in range(2)]
            qh = [sb(cx, "qh%d" % i, [128, T], BF16) for i in range(2)]
            kh = [sb(cx, "kh%d" % i, [128, T + L], BF16) for i in range(2)]
            vh = [sb(cx, "vh%d" % i, [128, 18, 128], BF16) for i in range(2)]
            oh = [sb(cx, "oh%d" % i, [128, T], BF16) for i in range(2)]
            E_ = [sb(cx, "E%d" % i, [128, 7, 128]) for i in range(2)]
            P_ = [sb(cx, "P%d" % i, [128, 7, 128], BF16) for i in range(2)]
            rd = [sb(cx, "rd%d" % i, [128, 128]) for i in range(2)]
            pSa = [ps(cx, "pSa%d" % i) for i in range(2)]
            pSb = [ps(cx, "pSb%d" % i) for i in range(2)]
            pden = [ps(cx, "pden%d" % i) for i in range(2)]
            pO = [ps(cx, "pO%d" % i) for i in range(2)]
            S.dma("sp", am_s[:], amask[:], reads=[amask], writes=[am_s])
            it = 0
            for h in range(NH):
                i = h % 2
                bi, q_, k_, v_, o_ = bias[i], qh[i], kh[i], vh[i], oh[i]
                bb_ = biasb[i]
                S.dma("sp", bi[:], rpbG.t[h], reads=[rpbG], writes=[bi])
                S.dma("sp", q_[:], qT_d.t[b, h], reads=[qT_d], writes=[q_])
                S.dma("sp", k_[:], kT_d.t[b, h], reads=[kT_d], writes=[k_])
                S.dma("sp", v_[:], v_d.t[b].rearrange("(c p) n -> p c n", p=128)[:, :, h * 128:(h + 1) * 128],
                      reads=[v_d], writes=[v_])
                S.op("dve", lambda e, bi=bi, bb_=bb_: e.scalar_tensor_tensor(
                    out=bb_[:], in0=bi[:], scalar=1.0, in1=am_s[:], op0=ALU.mult, op1=ALU.add),
                    reads=[am_s, bi], writes=[bb_], cost=4.7)
                S.op("dve", lambda e, bb_=bb_: e.tensor_scalar(out=bb_[:], in0=bb_[:], scalar1=1.0 / scale, scalar2=None,
                                                               op0=ALU.mult), reads=[], writes=[bb_], cost=2.4)
                for rp in range(16):
                    case = {0: 0, 1: 1, 14: 3, 15: 4}.get(rp, 2)
                    ks = min(max(2 * rp - 4, 0), 22) * 64
                    kofs = [ks + j * 128 for j in range(5)] + [T, T + 128]
                    j2 = it % 2
                    it += 1
                    a_, b_, d_, O_, e_, p_, r_ = pSa[j2], pSb[j2], pden[j2], pO[j2], E_[j2], P_[j2], rd[j2]

                    def mms(e, a_=a_, b_=b_, k_=k_, q_=q_, rp=rp, kofs=kofs, bb_=bb_, case=case):
                        ins = None
                        for j in range(7):
                            dst = a_[:, j * 128:(j + 1) * 128] if j < 4 else b_[:, (j - 4) * 128:(j - 3) * 128]
                            ins = e.matmul(dst, lhsT=k_[:, kofs[j]:kofs[j] + 128], rhs=q_[:, rp * 128:(rp + 1) * 128],
                                           start=True, stop=(j >= 5))
                            if j < 5:
                                ins = e.matmul(dst, lhsT=id_bf[:], rhs=bb_[:, case * 7 + j, :], start=False, stop=True)
                        return ins
                    S.op("pe", mms, reads=[k_, q_, bb_, id_bf], writes=[a_, b_], cost=1.0)
                    S.op("act", lambda e, a_=a_, p_=p_: e.activation(
                        out=p_[:, 0:4, :], in_=a_[:, 0:512].rearrange("p (j q) -> p j q", q=128), func=AF.Exp, scale=scale),
                        reads=[a_], writes=[p_], cost=0.6)
                    S.op("act", lambda e, b_=b_, p_=p_: e.activation(
                        out=p_[:, 4:7, :], in_=b_[:, 0:384].rearrange("p (j q) -> p j q", q=128), func=AF.Exp, scale=scale),
                        reads=[b_], writes=[p_], cost=0.5)

                    def mmo(e, d_=d_, O_=O_, p_=p_, v_=v_, kofs=kofs):
                        ins = None
                        for j in range(7):
                            e.matmul(d_[:, 0:128], lhsT=ones_bf[:], rhs=p_[:, j, :], start=(j == 0), stop=(j == 6))
                        for j in range(7):
                            ins = e.matmul(O_[:, 0:128], lhsT=v_[:, kofs[j] // 128, :], rhs=p_[:, j, :],
                                           start=(j == 0), stop=(j == 6))
                        return ins
                    S.op("pe", mmo, reads=[p_, v_, ones_bf], writes=[d_, O_])
                    S.op("dve", lambda e, d_=d_, r_=r_: e.reciprocal(out=r_[:], in_=d_[:, 0:128]), reads=[d_], writes=[r_])
                    S.op("dve", lambda e, O_=O_, r_=r_, o_=o_, rp=rp: e.tensor_tensor(
                        out=o_[:, rp * 128:(rp + 1) * 128], in0=O_[:, 0:128], in1=r_[:], op=ALU.mult),
                        reads=[O_, r_], writes=[o_])
                S.dma("sp", mixT_d.t[b, h], o_[:], reads=[o_], writes=[mixT_d])
            S.barrier()

    def stage_D1(b):
        TL = T + L
        with ExitStack() as cx:
            cw = sb(cx, "cw", [128, 12, 5])
            cb = sb(cx, "cb", [128, 12])
            xr = [sb(cx, "xr%d" % i, [128, TL]) for i in range(2)]
            xb = [sb(cx, "xb%d" % i, [128, TL], BF16) for i in range(2)]
            dg = [sb(cx, "dg%d" % i, [128, 5, 128], BF16) for i in range(2)]
            so = [sb(cx, "so%d" % i, [128, TL], BF16) for i in range(2)]
            tr = [sb(cx, "tr%d" % i, [128, 8, 128], BF16) for i in range(2)]
            pt = [ps(cx, "pt%d" % i, dt=BF16) for i in range(2)]
            pcv = [ps(cx, "pcv%d" % i) for i in range(3)]
            S.dma("sp", cw[:], convw[:], reads=[convw], writes=[cw])
            S.dma("sp", cb[:], convb[:], reads=[convb], writes=[cb])
            nt = 0
            ncv = 0
            for ch in range(12):
                x_, xb_, dg_, s_ = xr[ch % 2], xb[ch % 2], dg[ch % 2], so[ch % 2]
                S.dma("sp" if ch % 2 else "act", x_[:], xbcT_d.t[b, ch], reads=[xbcT_d], writes=[x_])
                S.op("dve", lambda e, x_=x_, xb_=xb_: e.tensor_copy(out=xb_[:], in_=x_[:]), reads=[x_], writes=[xb_], cost=1.5)

                def mkdg(e, dg_=dg_, ch=ch):
                    ins = None
                    for j in range(5):
                        ins = e.tensor_scalar(out=dg_[:, j, :], in0=idf_s[:], scalar1=cw[:, ch, j:j + 1], scalar2=None, op0=ALU.mult)
                    return ins
                S.op("dve", mkdg, reads=[idf_s, cw], writes=[dg_], cost=1.0)
                for c0 in range(0, TL, 512):
                    w_ = min(512, TL - c0)
                    s0, s1 = (0, T) if c0 < T else (T, TL)
                    pc = pcv[ncv % 3]
                    ncv += 1

                    def mmc(e, pc=pc, dg_=dg_, xb_=xb_, c0=c0, w_=w_, s0=s0, s1=s1):
                        e.matmul(pc[:, 0:w_], lhsT=dg_[:, 2, :], rhs=xb_[:, c0:c0 + w_], start=True, stop=False)
                        ins = None
                        for jj, j in enumerate((0, 1, 3, 4)):
                            off = j - 2
                            lo, hi = max(c0, s0 - off), min(c0 + w_, s1 - off)
                            ins = e.matmul(pc[:, lo - c0:hi - c0], lhsT=dg_[:, j, :], rhs=xb_[:, lo + off:hi + off],
                                           start=False, stop=(jj == 3))
                        return ins
                    S.op("pe", mmc, reads=[dg_, xb_], writes=[pc], cost=1.2)
                    S.op("act", lambda e, pc=pc, s_=s_, c0=c0, w_=w_, ch=ch: e.activation(
                        out=s_[:, c0:c0 + w_], in_=pc[:, 0:w_], func=AF.Silu, bias=cb[:, ch:ch + 1]),
                        reads=[pc, cb], writes=[s_], cost=0.6)
                if ch >= 8:
                    dst = BT_d if ch < 10 else CT_d
                    S.dma("sp", dst.t[b, ch % 2], s_[:], reads=[s_], writes=[dst])
                if ch < 10:
                    for c8 in range(0, 18, 8):
                        n = min(8, 18 - c8)
                        p_, t_ = pt[nt % 2], tr[nt % 2]
                        nt += 1

                        def tp(e, p_=p_, s_=s_, c8=c8, n=n):
                            ins = None
                            for i in range(n):
                                ins = e.transpose(p_[:, i * 128:(i + 1) * 128], s_[:, (c8 + i) * 128:(c8 + i + 1) * 128], id_bf[:])
                            return ins
                        S.op("pe", tp, reads=[s_, id_bf], writes=[p_])
                        S.op("act", lambda e, p_=p_, t_=t_, n=n: e.activation(
                            out=t_[:, 0:n, :], in_=p_[:, 0:n * 128].rearrange("p (i c) -> p i c", c=128), func=AF.Copy),
                            reads=[p_], writes=[t_])
                        if ch < 8:
                            dv = xs_tok_d.t[b, c8 * 128:(c8 + n) * 128, ch * 128:(ch + 1) * 128]
                            dres = xs_tok_d
                        else:
                            dv = B_tok_d.t[b, c8 * 128:(c8 + n) * 128, (ch - 8) * 128:(ch - 7) * 128]
                            dres = B_tok_d
                        S.dma("sp", dv.rearrange("(i p) n -> p i n", p=128), t_[:, 0:n, :], reads=[t_], writes=[dres])
            S.barrier()

    def stage_D2(b):
        with ExitStack() as cx:
            xs_t = sb(cx, "xs_t", [128, 18, 1024], BF16)
            dt_t = sb(cx, "dt_t", [128, 18, 32])
            a_t = sb(cx, "a_t", [128, 18, 32])
            Bt = sb(cx, "Bt", [128, 18, 256], BF16)
            BT = sb(cx, "BT", [128, 2, T + L], BF16)
            CT = sb(cx, "CT", [128, 2, T + L], BF16)
            tri = sb(cx, "tri", [128, 2, 128])
            mrep = sb(cx, "mrep", [128, 2, 512])
            Abc = sb(cx, "Abc", [128, 32])
            dsk = sb(cx, "dsk", [128, 1024])
            S.dma("sp", xs_t[:], xs_tok_d.t[b].rearrange("(c p) n -> p c n", p=128), reads=[xs_tok_d], writes=[xs_t])
            S.dma("sp", dt_t[:], dt_d.t[b].rearrange("(c p) n -> p c n", p=128), reads=[dt_d], writes=[dt_t])
            S.dma("sp", Bt[:], B_tok_d.t[b].rearrange("(c p) n -> p c n", p=128), reads=[B_tok_d], writes=[Bt])
            S.dma("sp", BT[:], BT_d.t[b].rearrange("g p t -> p g t"), reads=[BT_d], writes=[BT])
            S.dma("sp", CT[:], CT_d.t[b].rearrange("g p t -> p g t"), reads=[CT_d], writes=[CT])
            S.dma("sp", tri[:], tri_in.t.rearrange("d p l -> p d l"), reads=[tri_in], writes=[tri])
            S.dma("sp", mrep[:], mrep_in.t.rearrange("d p l -> p d l"), reads=[mrep_in], writes=[mrep])
            S.dma("sp", Abc[:], alog_bc[:], reads=[alog_bc], writes=[Abc])
            S.dma("sp", dsk[:], dskip_bc[:], reads=[dskip_bc], writes=[dsk])
            S.op("act", lambda e: e.activation(out=Abc[:], in_=Abc[:], func=AF.Exp), reads=[Abc], writes=[Abc])
            S.op("dve", lambda e: e.scalar_tensor_tensor(
                out=a_t[:], in0=dt_t[:], scalar=-1.0, in1=Abc[:].unsqueeze(1).to_broadcast([128, 18, 32]),
                op0=ALU.mult, op1=ALU.mult), reads=[dt_t, Abc], writes=[a_t])

            with ExitStack() as c1:
                hst = [sb(c1, "hst%d" % d, [128, 1024]) for d in range(2)]
                hbf = [sb(c1, "hbf%d" % i, [128, 1024], BF16) for i in range(2)]
                acs = [sb(c1, "acs%d" % i, [128, 32]) for i in range(2)]
                wd = [sb(c1, "wd%d" % i, [128, 32]) for i in range(2)]
                cdb = [sb(c1, "cdb%d" % i, [128, 32]) for i in range(2)]
                xw = [sb(c1, "xw%d" % i, [128, 1024], BF16) for i in range(2)]
                tmpS = [sb(c1, "tmpS%d" % i, [128, 1024]) for i in range(2)]
                pc = [ps(c1, "pc%d" % i) for i in range(2)]
                pi = [ps(c1, "pi%d" % i) for i in range(4)]
                n1 = 0
                for d in range(2):
                    S.op("dve", lambda e, d=d: e.memset(hst[d][:], 0.0), writes=[hst[d]])
                    order = [16, 17] + list(range(16)) if d == 0 else [17, 16] + list(range(15, -1, -1))
                    for c in order:
                        k = n1 % 2
                        n1 += 1
                        ac, w_, cd_, xw_, pc_, tm = acs[k], wd[k], cdb[k], xw[k], pc[k], tmpS[k]
                        if c < 16:
                            hb = hbf[k]
                            S.op("act", lambda e, hb=hb, d=d: e.activation(out=hb[:], in_=hst[d][:], func=AF.Copy),
                                 reads=[hst[d]], writes=[hb])
                            S.dma("sp", hT_d.t[b, d, c], hb[:], reads=[hb], writes=[hT_d])

                        def mmc(e, pc_=pc_, c=c, d=d):
                            e.matmul(pc_[:, 0:16], lhsT=tri[:, d, :], rhs=a_t[:, c, d * 16:(d + 1) * 16], start=True, stop=True)
                            return e.matmul(pc_[:, 32:48], lhsT=ones_f[:], rhs=a_t[:, c, d * 16:(d + 1) * 16],
                                            start=True, stop=True)
                        S.op("pe", mmc, reads=[tri, a_t, ones_f], writes=[pc_])
                        S.op("act", lambda e, pc_=pc_, ac=ac: e.activation(out=ac[:, 0:16], in_=pc_[:, 0:16], func=AF.Copy),
                             reads=[pc_], writes=[ac])
                        S.op("dve", lambda e, pc_=pc_, w_=w_, ac=ac: e.tensor_tensor(out=w_[:, 0:16], in0=pc_[:, 32:48],
                                                                                      in1=ac[:, 0:16], op=ALU.subtract),
                             reads=[pc_, ac], writes=[w_])
                        S.op("act", lambda e, w_=w_: e.activation(out=w_[:, 0:16], in_=w_[:, 0:16], func=AF.Exp),
                             reads=[w_], writes=[w_])
                        S.op("act", lambda e, pc_=pc_, cd_=cd_: e.activation(out=cd_[:, 0:16], in_=pc_[:, 32:48], func=AF.Exp),
                             reads=[pc_], writes=[cd_])
                        S.op("dve", lambda e, w_=w_, c=c, d=d: e.tensor_tensor(
                            out=w_[:, 0:16], in0=w_[:, 0:16], in1=dt_t[:, c, d * 16:(d + 1) * 16], op=ALU.mult),
                            reads=[dt_t], writes=[w_])
                        S.op("dve", lambda e, w_=w_, xw_=xw_, c=c: e.tensor_tensor(
                            out=xw_[:].rearrange("p (h q) -> p h q", q=64), in0=xs_t[:, c, :].rearrange("p (h q) -> p h q", q=64),
                            in1=w_[:, 0:16].unsqueeze(2).to_broadcast([128, 16, 64]), op=ALU.mult),
                            reads=[xs_t, w_], writes=[xw_])
                        for g in range(2):
                            pi_ = pi[(n1 * 2 + g) % 4]
                            S.op("pe", lambda e, pi_=pi_, c=c, g=g, xw_=xw_: e.matmul(
                                pi_[:], lhsT=Bt[:, c, g * 128:(g + 1) * 128], rhs=xw_[:, g * 512:(g + 1) * 512],
                                start=True, stop=True), reads=[Bt, xw_], writes=[pi_])
                            S.op("dve", lambda e, tm=tm, d=d, g=g, cd_=cd_: e.tensor_tensor(
                                out=tm[:, g * 512:(g + 1) * 512].rearrange("p (h q) -> p h q", q=64),
                                in0=hst[d][:, g * 512:(g + 1) * 512].rearrange("p (h q) -> p h q", q=64),
                                in1=cd_[:, g * 8:(g + 1) * 8].unsqueeze(2).to_broadcast([128, 8, 64]), op=ALU.mult),
                                reads=[hst[d], cd_], writes=[tm])
                            S.op("dve", lambda e, tm=tm, d=d, g=g, pi_=pi_: e.tensor_tensor(
                                out=hst[d][:, g * 512:(g + 1) * 512], in0=pi_[:], in1=tm[:, g * 512:(g + 1) * 512], op=ALU.add),
                                reads=[pi_, tm], writes=[hst[d]])
                S.barrier()

            with ExitStack() as c2:
                wn = sb(c2, "wn", [128, 1024])
                R_s = [sb(c2, "R_%d" % i, [128, 16, 128]) for i in range(2)]
                sg = [sb(c2, "sg%d" % d, [128, 16, 128]) for d in range(2)]
                MT = [sb(c2, "MT%d" % d, [128, 16, 128], BF16) for d in range(2)]
                xdt = [sb(c2, "xdt%d" % d, [128, 1024], BF16) for d in range(2)]
                hin = [sb(c2, "hin%d" % d, [128, 1024], BF16) for d in range(2)]
                acs2 = sb(c2, "acs2", [128, 32])
                eac = sb(c2, "eac", [128, 32])
                GT = sb(c2, "GT", [128, 2, 128])
                yac = sb(c2, "yac", [128, 1024])
                ytm = sb(c2, "ytm", [128, 1024])
                szt = sb(c2, "szt", [128, 1024])
                ss2 = sb(c2, "ss2", [128, 2])
                ybf = sb(c2, "ybf", [128, 1024], BF16)
                yT = sb(c2, "yT", [128, 8, 128], BF16)
                pacs = [ps(c2, "pacs%d" % i) for i in range(4)]
                pmisc = ps(c2, "pmisc")
                py = [ps(c2, "py%d" % i) for i in range(2)]
                ptb = ps(c2, "ptb", dt=BF16)
                S.dma("sp", wn[:], ssmw_bc[:], reads=[ssmw_bc], writes=[wn])
                for c in range(16):
                    S.dma("sp", szt[:], sz_d.t[b, c * 128:(c + 1) * 128, :], reads=[sz_d], writes=[szt])
                    def mm0(e, c=c):
                        e.matmul(pmisc[:, 0:16], lhsT=tri[:, 0, :], rhs=a_t[:, c, 0:16], start=True, stop=True)
                        e.matmul(pmisc[:, 16:32], lhsT=tri[:, 1, :], rhs=a_t[:, c, 16:32], start=True, stop=True)
                        ins = None
                        for g in range(2):
                            ins = e.matmul(pmisc[:, 128 + g * 128:256 + g * 128], lhsT=BT[:, g, c * 128:(c + 1) * 128],
                                           rhs=CT[:, g, c * 128:(c + 1) * 128], start=True, stop=True)
                        return ins
                    S.op("pe", mm0, reads=[tri, a_t, BT, CT], writes=[pmisc])
                    S.op("act", lambda e: e.activation(out=acs2[:], in_=pmisc[:, 0:32], func=AF.Copy, scale=-1.0), reads=[pmisc], writes=[acs2])
                    S.op("act", lambda e: e.activation(out=eac[:], in_=pmisc[:, 0:32], func=AF.Exp), reads=[pmisc], writes=[eac])
                    S.op("act", lambda e: e.activation(out=GT[:], in_=pmisc[:, 128:384].rearrange("p (g l) -> p g l", l=128),
                                                       func=AF.Copy), reads=[pmisc], writes=[GT])
                    for d in range(2):
                        R_ = R_s[d]
                        S.dma("sp", hin[d][:], hT_d.t[b, d, c], reads=[hT_d], writes=[hin[d]])
                        S.op("dve", lambda e, d=d, c=c: e.tensor_tensor(
                            out=xdt[d][:].rearrange("p (h q) -> p h q", q=64), in0=xs_t[:, c, :].rearrange("p (h q) -> p h q", q=64),
                            in1=dt_t[:, c, d * 16:(d + 1) * 16].unsqueeze(2).to_broadcast([128, 16, 64]), op=ALU.mult),
                            reads=[xs_t, dt_t], writes=[xdt[d]])
                        S.op("dve", lambda e, d=d, c=c: e.tensor_tensor(
                            out=R_[:], in0=tri[:, d, :].unsqueeze(1).to_broadcast([128, 16, 128]),
                            in1=a_t[:, c, d * 16:(d + 1) * 16].unsqueeze(2).to_broadcast([128, 16, 128]), op=ALU.mult),
                            reads=[tri, a_t], writes=[R_])
                        for q4 in range(4):
                            pa_ = pacs[q4]

                            def mma(e, pa_=pa_, q4=q4, d=d):
                                e.matmul(pa_[:], lhsT=ones_f[:], rhs=R_[:, q4 * 4:(q4 + 1) * 4, :].rearrange("p h l -> p (h l)"),
                                         start=True, stop=False)
                                return e.matmul(pa_[:], lhsT=idf_s[:], rhs=mrep[:, d, :], start=False, stop=True)
                            S.op("pe", mma, reads=[R_, ones_f, idf_s, mrep], writes=[pa_])
                            def expsub(e, pa_=pa_, q4=q4, d=d):
                                ins = None
                                for hh in range(4):
                                    h = q4 * 4 + hh
                                    ins = e.activation(out=sg[d][:, h, :], in_=pa_[:, hh * 128:(hh + 1) * 128], func=AF.Exp,
                                                       bias=acs2[:, d * 16 + h:d * 16 + h + 1])
                                return ins
                            S.op("act", expsub, reads=[pa_, acs2], writes=[sg[d]], cost=1.3)
                        for g in range(2):
                            S.op("dve", lambda e, d=d, g=g: e.tensor_tensor(
                                out=MT[d][:, g * 8:(g + 1) * 8, :], in0=sg[d][:, g * 8:(g + 1) * 8, :],
                                in1=GT[:, g, :].unsqueeze(1).to_broadcast([128, 8, 128]), op=ALU.mult),
                                reads=[sg[d], GT], writes=[MT[d]])

                    def mmy(e):
                        ins = None
                        for h in range(16):
                            dst = py[h // 8][:, (h % 8) * 64:(h % 8 + 1) * 64]
                            e.matmul(dst, lhsT=MT[0][:, h, :], rhs=xdt[0][:, h * 64:(h + 1) * 64], start=True, stop=False)
                            ins = e.matmul(dst, lhsT=MT[1][:, h, :], rhs=xdt[1][:, h * 64:(h + 1) * 64], start=False, stop=True)
                        return ins
                    S.op("pe", mmy, reads=[MT[0], MT[1], xdt[0], xdt[1]], writes=[py[0], py[1]])
                    S.op("dve", lambda e, c=c: e.tensor_tensor(out=yac[:], in0=xs_t[:, c, :], in1=dsk[:], op=ALU.mult),
                         reads=[xs_t, dsk], writes=[yac])
                    for g in range(2):
                        S.op("dve", lambda e, g=g: e.tensor_tensor(out=yac[:, g * 512:(g + 1) * 512], in0=py[g][:],
                                                                    in1=yac[:, g * 512:(g + 1) * 512], op=ALU.add),
                             reads=[py[g]], writes=[yac])
                    for d in range(2):
                        for g in range(2):
                            pyo = pacs[d * 2 + g]
                            S.op("pe", lambda e, d=d, g=g, c=c, pyo=pyo: e.matmul(
                                pyo[:], lhsT=CT[:, g, c * 128:(c + 1) * 128], rhs=hin[d][:, g * 512:(g + 1) * 512],
                                start=True, stop=True), reads=[CT, hin[d]], writes=[pyo])
                            S.op("dve", lambda e, d=d, g=g, pyo=pyo: e.tensor_tensor(
                                out=ytm[:, g * 512:(g + 1) * 512].rearrange("p (h q) -> p h q", q=64),
                                in0=pyo[:].rearrange("p (h q) -> p h q", q=64),
                                in1=eac[:, d * 16 + g * 8:d * 16 + g * 8 + 8].unsqueeze(2).to_broadcast([128, 8, 64]),
                                op=ALU.mult), reads=[pyo, eac], writes=[ytm])
                            S.op("dve", lambda e, g=g: e.tensor_tensor(
                                out=yac[:, g * 512:(g + 1) * 512], in0=yac[:, g * 512:(g + 1) * 512],
                                in1=ytm[:, g * 512:(g + 1) * 512], op=ALU.add), reads=[ytm], writes=[yac])
                    S.op("dve", lambda e: e.tensor_tensor(out=yac[:], in0=yac[:], in1=szt[:], op=ALU.mult),
                         reads=[szt], writes=[yac])
                    for g in range(2):
                        S.op("act", lambda e, g=g: e.activation(out=ytm[:, g * 512:(g + 1) * 512], in_=yac[:, g * 512:(g + 1) * 512],
                                                                func=AF.Square, accum_out=ss2[:, g:g + 1]),
                             reads=[yac], writes=[ytm, ss2])
                    S.op("dve", lambda e: e.tensor_scalar(out=ss2[:], in0=ss2[:], scalar1=1.0 / 512, scalar2=EPS,
                                                          op0=ALU.mult, op1=ALU.add), reads=[], writes=[ss2])
                    S.op("act", lambda e: e.activation(out=ss2[:], in_=ss2[:], func=AF.Sqrt), reads=[], writes=[ss2])
                    S.op("dve", lambda e: e.reciprocal(out=ss2[:], in_=ss2[:]), reads=[], writes=[ss2])
                    for g in range(2):
                        S.op("dve", lambda e, g=g: e.scalar_tensor_tensor(
                            out=ybf[:, g * 512:(g + 1) * 512], in0=yac[:, g * 512:(g + 1) * 512], scalar=ss2[:, g:g + 1],
                            in1=wn[:, g * 512:(g + 1) * 512], op0=ALU.mult, op1=ALU.mult),
                            reads=[yac, ss2, wn], writes=[ybf])

                    def tpy(e):
                        ins = None
                        for i in range(8):
                            ins = e.transpose(ptb[:, i * 128:(i + 1) * 128], ybf[:, i * 128:(i + 1) * 128], id_bf[:])
                        return ins
                    S.op("pe", tpy, reads=[ybf, id_bf], writes=[ptb])
                    S.op("act", lambda e: e.activation(out=yT[:], in_=ptb[:, 0:1024].rearrange("p (i c) -> p i c", c=128),
                                                       func=AF.Copy), reads=[ptb], writes=[yT])
                    S.dma("sp", mixT_d.t[b, 8:16, :, c * 128:(c + 1) * 128].rearrange("i p t -> p i t"), yT[:],
                          reads=[yT], writes=[mixT_d])
                S.barrier()

    def stage_E(b):
        with ExitStack() as cx:
            lg = sb(cx, "lg", [NE, T])
            with ExitStack() as c1:
                mixTs = [sb(c1, "mixT%d" % i, [128, KC, 512], BF16) for i in range(2)]
                wo = [sb(c1, "wo%d" % i, [128, KC, 512], BF16) for i in range(3)]
                h1s = [sb(c1, "h1_%d" % i, [128, KC, 512]) for i in range(2)]
                u2bs = [sb(c1, "u2b%d" % i, [128, KC, 512], BF16) for i in range(2)]
                xt = [sb(c1, "xt%d" % i, [128, 512]) for i in range(2)]
                u2f = [sb(c1, "u2f%d" % i, [128, 512]) for i in range(2)]
                sqb = [sb(c1, "sqb%d" % i, [128, 512], BF16) for i in range(2)]
                rs_ = sb(c1, "rs_", [128, 512])
                wr = sb(c1, "wr", [128, KC, NE])
                trs = [sb(c1, "trs%d" % i, [128, 1024], BF16) for i in range(2)]
                pmm = [ps(c1, "pmm%d" % i) for i in range(3)]
                pss = ps(c1, "pssE")
                plg = ps(c1, "plg")
                ptr = [ps(c1, "ptr%d" % i, dt=BF16) for i in range(2)]
                S.dma("sp", wr[:], w_rT[:], reads=[w_rT], writes=[wr])
                wov = w_out.t.rearrange("(kc p) n -> p kc n", p=128)
                n = {"w": 0, "pm": 0, "x": 0, "tr": 0}
                for tt in range(4):
                    t0 = tt * 512
                    mixT, h1, u2b = mixTs[tt % 2], h1s[tt % 2], u2bs[tt % 2]
                    S.dma("act", mixT[:], mixT_d.t[b, :, :, t0:t0 + 512].rearrange("c p t -> p c t"), reads=[mixT_d], writes=[mixT])
                    for gq in range(4):
                        w = wo[n["w"] % 3]
                        n["w"] += 1
                        S.dma("pool", w[:], wov[:, :, gq * 512:(gq + 1) * 512], reads=[w_out], writes=[w])
                        for jj in range(4):
                            j = gq * 4 + jj
                            p_ = pmm[n["pm"] % 3]
                            n["pm"] += 1
                            x_ = xt[n["x"] % 2]
                            sq_ = sqb[n["x"] % 2]
                            n["x"] += 1
                            S.dma("sp", x_[:], xT.t[b, j * 128:(j + 1) * 128, t0:t0 + 512], reads=[xT], writes=[x_])

                            def mmo(e, w=w, p_=p_, jj=jj, t0=t0):
                                ins = None
                                for kc in range(KC):
                                    ins = e.matmul(p_[:], lhsT=w[:, kc, jj * 128:(jj + 1) * 128], rhs=mixT[:, kc, :],
                                                   start=(kc == 0), stop=(kc == KC - 1))
                                return ins
                            S.op("pe", mmo, reads=[w, mixT], writes=[p_])
                            S.op("dve", lambda e, p_=p_, x_=x_, j=j: e.scalar_tensor_tensor(
                                out=h1[:, j, :], in0=p_[:], scalar=modT[:, G1 + j, b:b + 1], in1=x_[:],
                                op0=ALU.mult, op1=ALU.add), reads=[p_, x_, modT], writes=[h1])
                            S.dma("sp", h1T_d.t[b, j, :, t0:t0 + 512], h1[:, j, :], reads=[h1], writes=[h1T_d])
                            S.op("act", lambda e, sq_=sq_, j=j: e.activation(out=sq_[:], in_=h1[:, j, :], func=AF.Square),
                                 reads=[h1], writes=[sq_])
                            S.op("pe", lambda e, sq_=sq_, j=j: e.matmul(pss[:], lhsT=ones_bf[:], rhs=sq_[:],
                                                                         start=(j == 0), stop=(j == KC - 1)),
                                 reads=[sq_, ones_bf], writes=[pss])
                    S.op("dve", lambda e: e.tensor_scalar(out=rs_[:], in0=pss[:], scalar1=1.0 / D, scalar2=EPS,
                                                          op0=ALU.mult, op1=ALU.add), reads=[pss], writes=[rs_])
                    S.op("act", lambda e: e.activation(out=rs_[:], in_=rs_[:], func=AF.Sqrt), reads=[], writes=[rs_])
                    S.op("dve", lambda e: e.reciprocal(out=rs_[:], in_=rs_[:]), reads=[], writes=[rs_])
                    for j in range(KC):
                        uf = u2f[j % 2]
                        S.op("dve", lambda e, uf=uf, j=j: e.scalar_tensor_tensor(
                            out=uf[:], in0=h1[:, j, :], scalar=A2[:, j, b:b + 1], in1=rs_[:], op0=ALU.mult, op1=ALU.mult),
                            reads=[h1, rs_, A2], writes=[uf])
                        S.op("act", lambda e, uf=uf, j=j: e.activation(out=uf[:], in_=uf[:], func=AF.Identity,
                                                                        bias=modT[:, SH2 + j, b:b + 1]),
                             reads=[modT], writes=[uf])
                        S.op("pe", lambda e, uf=uf, j=j: e.matmul(plg[0:NE, :], lhsT=wr[:, j, :], rhs=uf[:],
                                                                   start=(j == 0), stop=(j == KC - 1)),
                             reads=[uf, wr], writes=[plg])
                        S.op("act", lambda e, uf=uf, j=j: e.activation(out=u2b[:, j, :], in_=uf[:], func=AF.Copy),
                             reads=[uf], writes=[u2b])
                    S.op("act", lambda e, t0=t0: e.activation(out=lg[:, t0:t0 + 512], in_=plg[0:NE, :], func=AF.Exp),
                         reads=[plg], writes=[lg])
                    for tc_ in range(4):
                        for hf in range(2):
                            pt_ = ptr[n["tr"] % 2]
                            ts_ = trs[n["tr"] % 2]
                            n["tr"] += 1

                            def tpu(e, pt_=pt_, tc_=tc_, hf=hf):
                                ins = None
                                for i in range(8):
                                    ins = e.transpose(pt_[:, i * 128:(i + 1) * 128],
                                                      u2b[:, hf * 8 + i, tc_ * 128:(tc_ + 1) * 128], id_bf[:])
                                return ins
                            S.op("pe", tpu, reads=[u2b, id_bf], writes=[pt_])
                            S.op("act", lambda e, pt_=pt_, ts_=ts_: e.activation(out=ts_[:], in_=pt_[:], func=AF.Copy),
                                 reads=[pt_], writes=[ts_])
                            r0 = t0 + tc_ * 128
                            S.dma("sp", u2tok_ds[b].t[r0:r0 + 128, hf * 1024:(hf + 1) * 1024], ts_[:], reads=[ts_], writes=[u2tok_ds[b]])
                S.barrier()
            with ExitStack() as c2:
                aff = sb(c2, "aff", [NE, T])
                psm = ps(c2, "psm")
                for tt in range(4):
                    sl = slice(tt * 512, (tt + 1) * 512)
                    S.op("pe", lambda e, sl=sl: e.matmul(psm[0:NE, :], lhsT=ones_f[0:NE, 0:NE], rhs=lg[:, sl], start=True, stop=True),
                         reads=[lg, ones_f], writes=[psm])
                    S.op("dve", lambda e, sl=sl: e.reciprocal(out=aff[:, sl], in_=psm[0:NE, :]), reads=[psm], writes=[aff])
                    S.op("dve", lambda e, sl=sl: e.tensor_tensor(out=aff[:, sl], in0=aff[:, sl], in1=lg[:, sl], op=ALU.mult),
                         reads=[lg], writes=[aff])
                S.dma("sp", aff_d.t[b], aff[:], reads=[aff], writes=[aff_d])
                S.barrier()

    def stage_E2():
        PP = 32 * (nb - 1) + NE
        with ExitStack() as c2:
            aff = sb(c2, "affJ", [PP, T])
            W_ = sb(c2, "W_", [PP, T])
            m8 = sb(c2, "m8", [PP, 8])
            tau = sb(c2, "tau", [PP, 1])
            msk = sb(c2, "msk", [PP, T])
            gt = sb(c2, "gt", [PP, T])
            sc0 = sb(c2, "sc0", [PP, T])
            sc1 = sb(c2, "sc1", [PP, T])
            pbf = sb(c2, "pbf", [PP, T], BF16)
            pTs = [sb(c2, "pT%d" % i, [128, 256]) for i in range(nb)]
            ptps = [ps(c2, "ptp%d" % i, dt=BF16) for i in range(nb)]
            S.op("dve", lambda e: e.memset(aff[:], 0.0), writes=[aff])
            for bb in range(nb):
                S.dma("sp", aff[32 * bb:32 * bb + NE, :], aff_d.t[bb], reads=[aff_d], writes=[aff])
            S.op("dve", lambda e: e.tensor_copy(out=W_[:], in_=aff[:]), reads=[aff], writes=[W_])
            for r_ in range(CAP // 8):
                S.op("dve", lambda e: e.max(out=m8[:], in_=W_[:]), reads=[W_], writes=[m8], cost=2.2)
                if r_ < CAP // 8 - 1:
                    S.op("dve", lambda e: e.match_replace(out=W_[:], in_to_replace=m8[:], in_values=W_[:], imm_value=-1.0),
                         reads=[m8], writes=[W_], cost=2.2)
            S.op("dve", lambda e: e.tensor_reduce(out=tau[:], in_=m8[:], axis=mybir.AxisListType.X, op=ALU.min),
                 reads=[m8], writes=[tau])
            S.op("dve", lambda e: e.tensor_scalar(out=msk[:], in0=aff[:], scalar1=tau[:, 0:1], scalar2=None, op0=ALU.is_ge),
                 reads=[aff, tau], writes=[msk])
            S.op("dve", lambda e: e.tensor_tensor(out=gt[:], in0=aff[:], in1=msk[:], op=ALU.mult), reads=[aff, msk], writes=[gt])
            for bb in range(nb):
                S.dma("sp", gate_d.t[bb], gt[32 * bb:32 * bb + NE, :], reads=[gt], writes=[gate_d])
            S.op("dve", lambda e: e.tensor_copy(out=sc0[:], in_=msk[:]), reads=[msk], writes=[sc0])
            cur, nxt = sc0, sc1
            k = 1
            while k < T:
                S.op("dve", lambda e, cur=cur, nxt=nxt, k=k: e.tensor_copy(out=nxt[:, 0:k], in_=cur[:, 0:k]),
                     reads=[cur], writes=[nxt])
                S.op("dve", lambda e, cur=cur, nxt=nxt, k=k: e.tensor_tensor(out=nxt[:, k:T], in0=cur[:, k:T], in1=cur[:, 0:T - k],
                                                                             op=ALU.add), reads=[cur], writes=[nxt])
                cur, nxt = nxt, cur
                k *= 2
            S.op("dve", lambda e, cur=cur, nxt=nxt: e.tensor_tensor(out=nxt[:], in0=cur[:], in1=msk[:], op=ALU.mult),
                 reads=[cur, msk], writes=[nxt])
            S.op("dve", lambda e, nxt=nxt: e.tensor_scalar(out=pbf[:], in0=nxt[:], scalar1=-1.0, scalar2=None, op0=ALU.add),
                 reads=[nxt], writes=[pbf])
            for bb in range(nb):
                p0 = 32 * bb
                S.dma("sp", pos_d.t[bb], pbf[p0:p0 + NE, :], reads=[pbf], writes=[pos_d])
                ptp, pT = ptps[bb], pTs[bb]

                def tpp(e, ptp=ptp, p0=p0):
                    ins = None
                    for tc_ in range(16):
                        ins = e.transpose(ptp[:, tc_ * 16:(tc_ + 1) * 16], pbf[p0:p0 + NE, tc_ * 128:(tc_ + 1) * 128],
                                          id_bf[p0:p0 + NE, p0:p0 + NE])
                    return ins
                S.op("pe", tpp, reads=[pbf, id_bf], writes=[ptp])
                S.op("act", lambda e, ptp=ptp, pT=pT: e.activation(out=pT[:], in_=ptp[:, 0:256], func=AF.Copy), reads=[ptp], writes=[pT])
                S.dma("sp", posT_d.t[bb], pT[:], reads=[pT], writes=[posT_d])
            S.barrier()

    def stage_F(b):
        I32 = mybir.dt.int32
        with ExitStack() as cx:
            pT = sb(cx, "pTf", [128, 16, NE])
            iota = sb(cx, "iota", [128, 256])
            tkf = sb(cx, "tkf", [128, 16, 2])
            tkb = sb(cx, "tkb", [128, 16, 2], BF16)
            sel = [sb(cx, "sel%d" % i, [128, 16, 256], BF16) for i in range(3)]
            idi = [sb(cx, "idi%d" % i, [128, 2], I32) for i in range(3)]
            Xs = [sb(cx, "Xs%d" % i, [128, D], BF16) for i in range(4)]
            XT = [sb(cx, "XTf%d" % i, [128, KC, 256], BF16) for i in range(2)]
            pix = [ps(cx, "pix%d" % i) for i in range(2)]
            ptx = [ps(cx, "ptx%d" % i, dt=BF16) for i in range(4)]
            S.dma("sp", pT[:], posT_d.t[b].rearrange("p (c e) -> p c e", e=NE), reads=[posT_d], writes=[pT])
            S.dma("sp", iota[:], iota_in[:], reads=[iota_in], writes=[iota])
            S.dma("sp", tkf[:], tokid_in[:], reads=[tokid_in], writes=[tkf])
            S.op("dve", lambda e: e.tensor_copy(out=tkb[:], in_=tkf[:]), reads=[tkf], writes=[tkb])
            nx = 0
            npt = 0
            for ex in range(NE):
                sl_, id_, px = sel[ex % 3], idi[ex % 3], pix[ex % 2]
                xt_ = XT[ex % 2]

                def mksel(e, sl_=sl_, ex=ex):
                    ins = None
                    for tc_ in range(16):
                        ins = e.tensor_scalar(out=sl_[:, tc_, :], in0=iota[:], scalar1=pT[:, tc_, ex:ex + 1], scalar2=None,
                                              op0=ALU.is_equal)
                    return ins
                S.op("dve", mksel, reads=[iota, pT], writes=[sl_], cost=5.0)

                def mmi(e, sl_=sl_, px=px):
                    ins = None
                    for h in range(2):
                        for tc_ in range(16):
                            ins = e.matmul(px[:, h * 2:h * 2 + 2], lhsT=sl_[:, tc_, h * 128:(h + 1) * 128], rhs=tkb[:, tc_, :],
                                           start=(tc_ == 0), stop=(tc_ == 15))
                    return ins
                S.op("pe", mmi, reads=[sl_, tkb], writes=[px], cost=2.5)
                S.op("dve", lambda e, px=px, id_=id_: e.tensor_scalar(
                    out=id_[:].rearrange("p (h o) -> p h o", o=1), in0=px[:, 0:4].rearrange("p (h t) -> p h t", t=2)[:, :, 0:1],
                    scalar1=128.0, scalar2=None, op0=ALU.mult), reads=[px], writes=[id_], cost=0.2)
                S.op("dve", lambda e, px=px, id_=id_: e.tensor_tensor(
                    out=id_[:].rearrange("p (h o) -> p h o", o=1), in0=id_[:].rearrange("p (h o) -> p h o", o=1),
                    in1=px[:, 0:4].rearrange("p (h t) -> p h t", t=2)[:, :, 1:2], op=ALU.add), reads=[px], writes=[id_], cost=0.2)
                for h in range(2):
                    x_ = Xs[nx % 4]
                    nx += 1
                    S.idma("pool", lambda e, x_=x_, id_=id_, h=h: e.indirect_dma_start(
                        out=x_[:, :], out_offset=None, in_=u2tok_ds[b].t[:, :],
                        in_offset=bass.IndirectOffsetOnAxis(ap=id_[:, h:h + 1], axis=0)),
                        reads=[id_, u2tok_ds[b]], writes=[x_], nbytes=128 * D * 2)
                    for k8 in range(2):
                        pt_ = ptx[npt % 4]
                        npt += 1

                        def tpx(e, pt_=pt_, x_=x_, k8=k8):
                            ins = None
                            for i in range(8):
                                kc = k8 * 8 + i
                                ins = e.transpose(pt_[:, i * 128:(i + 1) * 128], x_[:, kc * 128:(kc + 1) * 128], id_bf[:])
                            return ins
                        S.op("pe", tpx, reads=[x_, id_bf], writes=[pt_], cost=1.5)
                        S.op("act", lambda e, pt_=pt_, xt_=xt_, k8=k8, h=h: e.activation(
                            out=xt_[:, k8 * 8:(k8 + 1) * 8, h * 128:(h + 1) * 128],
                            in_=pt_[:, 0:1024].rearrange("p (i s) -> p i s", s=128), func=AF.Copy),
                            reads=[pt_], writes=[xt_], cost=0.9)
                S.dma("sp", XselT_d.t[ex, :, :, b * CAP:(b + 1) * CAP].rearrange("c p t -> p c t"), xt_[:],
                      reads=[xt_], writes=[XselT_d], nbytes=128 * KC * 256 * 2)
            S.barrier()

    def stage_G():
        NT = nb * CAP
        I32 = mybir.dt.int32
        with ExitStack() as cx:
            XT = [sb(cx, "XT%d" % i, [128, KC, NT], BF16) for i in range(2)]
            wg = [sb(cx, "wg%d" % i, [128, KC, 256], BF16) for i in range(3)]
            wu = [sb(cx, "wu%d" % i, [128, KC, 256], BF16) for i in range(3)]
            wd = [sb(cx, "wd%d" % i, [128, FC, 512], BF16) for i in range(2)]
            hid = sb(cx, "hid", [128, FC, NT], BF16)
            sg_ = [sb(cx, "sgG%d" % i, [128, NT]) for i in range(2)]
            yo = [sb(cx, "yo%d" % i, [128, 512], BF16) for i in range(3)]
            pgs = [ps(cx, "pgs%d" % i) for i in range(2)]
            pus = [ps(cx, "pus%d" % i) for i in range(2)]
            pds = [ps(cx, "pds%d" % i) for i in range(2)]
            pTs = [sb(cx, "pTg%d" % i, [128, 16, NE]) for i in range(nb)]
            iota = sb(cx, "iotaG", [128, 256])
            tkf = sb(cx, "tkf", [128, 16, 2])
            tkb = sb(cx, "tkb", [128, 16, 2], BF16)
            sel = [sb(cx, "sel%d" % i, [128, 16, 256], BF16) for i in range(2)]
            idi = [sb(cx, "idi%d" % i, [128, 2], I32) for i in range(3)]
            Xs = [sb(cx, "Xs%d" % i, [128, D], BF16) for i in range(3)]
            pix = ps(cx, "pix")
            ptx = ps(cx, "ptx", dt=BF16)
            for bb in range(nb):
                S.dma("sp", pTs[bb][:], posT_d.t[bb].rearrange("p (c e) -> p c e", e=NE), reads=[posT_d], writes=[pTs[bb]])
            S.dma("sp", iota[:], iota_in[:], reads=[iota_in], writes=[iota])
            S.dma("sp", tkf[:], tokid_in[:], reads=[tokid_in], writes=[tkf])
            S.op("dve", lambda e: e.tensor_copy(out=tkb[:], in_=tkf[:]), reads=[tkf], writes=[tkb])
            n = {"w": 0, "p": 0, "d": 0, "y": 0, "s": 0, "x": 0}

            def gather(ex):
                xt_ = XT[ex % 2]
                for bb in range(nb):
                    sl_, id_ = sel[n["s"] % 2], idi[n["s"] % 3]
                    n["s"] += 1

                    def mksel(e, sl_=sl_, ex=ex, bb=bb):
                        ins = None
                        for tc_ in range(16):
                            ins = e.tensor_scalar(out=sl_[:, tc_, :], in0=iota[:], scalar1=pTs[bb][:, tc_, ex:ex + 1], scalar2=None,
                                                  op0=ALU.is_equal)
                        return ins
                    S.op("dve", mksel, reads=[iota, pTs[bb]], writes=[sl_], cost=5.0)

                    def mmi(e, sl_=sl_):
                        ins = None
                        for h in range(2):
                            for tc_ in range(16):
                                ins = e.matmul(pix[:, h * 2:h * 2 + 2], lhsT=sl_[:, tc_, h * 128:(h + 1) * 128], rhs=tkb[:, tc_, :],
                                               start=(tc_ == 0), stop=(tc_ == 15))
                        return ins
                    S.op("pe", mmi, reads=[sl_, tkb], writes=[pix], cost=2.5)
                    S.op("dve", lambda e, id_=id_: e.tensor_scalar(
                        out=id_[:].rearrange("p (h o) -> p h o", o=1), in0=pix[:, 0:4].rearrange("p (h t) -> p h t", t=2)[:, :, 0:1],
                        scalar1=128.0, scalar2=None, op0=ALU.mult), reads=[pix], writes=[id_], cost=0.2)
                    S.op("dve", lambda e, id_=id_: e.tensor_tensor(
                        out=id_[:].rearrange("p (h o) -> p h o", o=1), in0=id_[:].rearrange("p (h o) -> p h o", o=1),
                        in1=pix[:, 0:4].rearrange("p (h t) -> p h t", t=2)[:, :, 1:2], op=ALU.add), reads=[pix], writes=[id_], cost=0.2)
                    for h in range(2):
                        x_ = Xs[n["x"] % 3]
                        n["x"] += 1
                        S.idma("pool", lambda e, x_=x_, id_=id_, h=h, bb=bb: e.indirect_dma_start(
                            out=x_[:, :], out_offset=None, in_=u2tok_ds[bb].t[:, :],
                            in_offset=bass.IndirectOffsetOnAxis(ap=id_[:, h:h + 1], axis=0)),
                            reads=[id_, u2tok_ds[bb]], writes=[x_], nbytes=128 * D * 2)
                        for k8 in range(2):
                            def tpx(e, x_=x_, k8=k8):
                                ins = None
                                for i in range(8):
                                    kc = k8 * 8 + i
                                    ins = e.transpose(ptx[:, i * 128:(i + 1) * 128], x_[:, kc * 128:(kc + 1) * 128], id_bf[:])
                                return ins
                            S.op("pe", tpx, reads=[x_, id_bf], writes=[ptx], cost=1.5)
                            c0 = bb * CAP + h * 128
                            S.op("act", lambda e, xt_=xt_, k8=k8, c0=c0: e.activation(
                                out=xt_[:, k8 * 8:(k8 + 1) * 8, c0:c0 + 128],
                                in_=ptx[:, 0:1024].rearrange("p (i s) -> p i s", s=128), func=AF.Copy),
                                reads=[ptx], writes=[xt_], cost=0.9)
                if debug:
                    S.dma("sp", XselT_d.t[ex].rearrange("c p t -> p c t"), xt_[:], reads=[xt_], writes=[XselT_d])

            gather(0)
            for ex in range(NE):
                X_ = XT[ex % 2]
                gv = w_gate.t[ex].rearrange("(kc p) f -> p kc f", p=128)
                uv = w_up.t[ex].rearrange("(kc p) f -> p kc f", p=128)
                dv = w_down.t[ex].rearrange("(fc p) d -> p fc d", p=128)
                for fg in range(11):
                    ncol = 256
                    g_, u_ = wg[n["w"] % 3], wu[n["w"] % 3]
                    n["w"] += 1
                    S.dma("pool", g_[:, :, 0:ncol], gv[:, :, fg * 256:fg * 256 + ncol], reads=[w_gate], writes=[g_], nbytes=128 * KC * 256 * 4)
                    S.dma("pool", u_[:, :, 0:ncol], uv[:, :, fg * 256:fg * 256 + ncol], reads=[w_up], writes=[u_], nbytes=128 * KC * 256 * 4)
                    if fg == 4 and ex + 1 < NE:
                        gather(ex + 1)
                    for fj in range(ncol // 128):
                        f = fg * 2 + fj
                        pg, pu = pgs[n["p"] % 2], pus[n["p"] % 2]
                        s_ = sg_[n["p"] % 2]
                        n["p"] += 1

                        def mmgu(e, g_=g_, u_=u_, pg=pg, pu=pu, fj=fj, X_=X_):
                            ins = None
                            for kc in range(KC):
                                e.matmul(pg[:, 0:NT], lhsT=g_[:, kc, fj * 128:(fj + 1) * 128], rhs=X_[:, kc, :],
                                         start=(kc == 0), stop=(kc == KC - 1))
                            for kc in range(KC):
                                ins = e.matmul(pu[:, 0:NT], lhsT=u_[:, kc, fj * 128:(fj + 1) * 128], rhs=X_[:, kc, :],
                                               start=(kc == 0), stop=(kc == KC - 1))
                            return ins
                        S.op("pe", mmgu, reads=[g_, u_, X_], writes=[pg, pu], cost=32 * NT / 2200.0)
                        S.op("act", lambda e, pg=pg, s_=s_: e.activation(out=s_[:], in_=pg[:, 0:NT], func=AF.Silu),
                             reads=[pg], writes=[s_], cost=0.6)
                        S.op("dve", lambda e, pu=pu, s_=s_, f=f: e.tensor_tensor(out=hid[:, f, :], in0=pu[:, 0:NT], in1=s_[:],
                                                                                  op=ALU.mult), reads=[pu, s_], writes=[hid], cost=0.7)
                for dg in range(4):
                    d_ = wd[n["d"] % 2]
                    n["d"] += 1
                    S.dma("pool", d_[:], dv[:, :, dg * 512:(dg + 1) * 512], reads=[w_down], writes=[d_], nbytes=128 * FC * 512 * 4)
                    for sc in range(NT // 128):
                        p_ = pds[n["y"] % 2]
                        o_ = yo[n["y"] % 3]
                        n["y"] += 1

                        def mmd(e, p_=p_, d_=d_, sc=sc):
                            ins = None
                            for f in range(FC):
                                ins = e.matmul(p_[:], lhsT=hid[:, f, sc * 128:(sc + 1) * 128], rhs=d_[:, f, :],
                                               start=(f == 0), stop=(f == FC - 1))
                            return ins
                        S.op("pe", mmd, reads=[hid, d_], writes=[p_], cost=FC * 512 / 2200.0)
                        S.op("act", lambda e, p_=p_, o_=o_: e.activation(out=o_[:], in_=p_[:], func=AF.Copy),
                             reads=[p_], writes=[o_], cost=0.6)
                        bb, hh = sc // 2, sc % 2
                        S.dma("sp", Y_d.t[bb, ex, hh * 128:(hh + 1) * 128, dg * 512:(dg + 1) * 512], o_[:],
                              reads=[o_], writes=[Y_d], nbytes=128 * 512 * 2)
            S.barrier()

    def stage_H(b):
        with ExitStack() as cx:
            posb = sb(cx, "posb", [NE, T], BF16)
            gts = sb(cx, "gts", [NE, T])
            selr_f = sb(cx, "selr_f", [NE, NE, 128])
            selr_b = sb(cx, "selr_b", [NE, NE, 128], BF16)
            jcol = sb(cx, "jcol", [128, 2])
            SGs = [sb(cx, "SG%d" % i, [128, 32, 512], BF16) for i in range(2)]
            gbc = [sb(cx, "gbc%d" % i, [128, 512]) for i in range(2)]
            Yj = [sb(cx, "Yj%d" % i, [128, 32, 128], BF16) for i in range(4)]
            h1t = [sb(cx, "h1t%d" % i, [128, 512]) for i in range(3)]
            h2 = sb(cx, "h2", [128, KC, 512])
            sqh = [sb(cx, "sqh%d" % i, [128, 512], BF16) for i in range(3)]
            rsh = sb(cx, "rsh", [128, 512])
            oo = [sb(cx, "oo%d" % i, [128, 512]) for i in range(3)]
            ppos = [ps(cx, "ppos%d" % i) for i in range(2)]
            pgat = [ps(cx, "pgat%d" % i) for i in range(2)]
            pacc = [ps(cx, "pacc%d" % i) for i in range(2)]
            pssH = ps(cx, "pssH")
            S.dma("sp", posb[:], pos_d.t[b], reads=[pos_d], writes=[posb])
            S.dma("sp", gts[:], gate_d.t[b], reads=[gate_d], writes=[gts])
            S.dma("sp", selr_f[:], selrow_in[:], reads=[selrow_in], writes=[selr_f])
            S.dma("sp", jcol[:], jcol_in[:], reads=[jcol_in], writes=[jcol])
            S.op("dve", lambda e: e.tensor_copy(out=selr_b[:], in_=selr_f[:]), reads=[selr_f], writes=[selr_b])
            n = {"e": 0, "j": 0}
            for tt in range(4):
                t0 = tt * 512
                SG = SGs[tt % 2]
                for ex in range(NE):
                    pp, pg, gb = ppos[n["e"] % 2], pgat[n["e"] % 2], gbc[n["e"] % 2]
                    n["e"] += 1
                    S.op("pe", lambda e, pp=pp, ex=ex, t0=t0: e.matmul(pp[:], lhsT=selr_b[:, ex, :], rhs=posb[:, t0:t0 + 512],
                                                                       start=True, stop=True),
                         reads=[selr_b, posb], writes=[pp])
                    S.op("pe", lambda e, pg=pg, ex=ex, t0=t0: e.matmul(pg[:], lhsT=selr_f[:, ex, :], rhs=gts[:, t0:t0 + 512],
                                                                       start=True, stop=True),
                         reads=[selr_f, gts], writes=[pg])
                    S.op("act", lambda e, pg=pg, gb=gb: e.activation(out=gb[:], in_=pg[:], func=AF.Copy), reads=[pg], writes=[gb])
                    for hh in range(2):
                        S.op("dve", lambda e, pp=pp, gb=gb, ex=ex, hh=hh: e.scalar_tensor_tensor(
                            out=SG[:, ex * 2 + hh, :], in0=pp[:], scalar=jcol[:, hh:hh + 1], in1=gb[:],
                            op0=ALU.is_equal, op1=ALU.mult), reads=[pp, gb, jcol], writes=[SG])
                for j in range(KC):
                    y_ = Yj[n["j"] % 4]
                    ht = h1t[n["j"] % 3]
                    sq_ = sqh[n["j"] % 3]
                    pa_ = pacc[n["j"] % 2]
                    n["j"] += 1
                    S.dma("sp" if n["j"] % 2 else "act", y_[:],
                          Y_d.t[b, :, :, j * 128:(j + 1) * 128].rearrange("e (h p) d -> p (e h) d", p=128),
                          reads=[Y_d], writes=[y_])
                    S.dma("sp", ht[:], h1T_d.t[b, j, :, t0:t0 + 512], reads=[h1T_d], writes=[ht])

                    def mms(e, y_=y_, pa_=pa_):
                        ins = None
                        for k in range(32):
                            ins = e.matmul(pa_[:], lhsT=y_[:, k, :], rhs=SG[:, k, :], start=(k == 0), stop=(k == 31))
                        return ins
                    S.op("pe", mms, reads=[y_, SG], writes=[pa_])
                    S.op("dve", lambda e, pa_=pa_, ht=ht, j=j: e.scalar_tensor_tensor(
                        out=h2[:, j, :], in0=pa_[:], scalar=modT[:, G2 + j, b:b + 1], in1=ht[:], op0=ALU.mult, op1=ALU.add),
                        reads=[pa_, ht, modT], writes=[h2])
                    S.op("act", lambda e, sq_=sq_, j=j: e.activation(out=sq_[:], in_=h2[:, j, :], func=AF.Square),
                         reads=[h2], writes=[sq_])
                    S.op("pe", lambda e, sq_=sq_, j=j: e.matmul(pssH[:], lhsT=ones_bf[:], rhs=sq_[:], start=(j == 0), stop=(j == KC - 1)),
                         reads=[sq_, ones_bf], writes=[pssH])
                S.op("dve", lambda e: e.tensor_scalar(out=rsh[:], in0=pssH[:], scalar1=1.0 / D, scalar2=EPS, op0=ALU.mult, op1=ALU.add),
                     reads=[pssH], writes=[rsh])
                S.op("act", lambda e: e.activation(out=rsh[:], in_=rsh[:], func=AF.Sqrt), reads=[], writes=[rsh])
                S.op("dve", lambda e: e.reciprocal(out=rsh[:], in_=rsh[:]), reads=[], writes=[rsh])
                for j in range(KC):
                    o_ = oo[j % 3]
                    S.op("dve", lambda e, o_=o_, j=j: e.scalar_tensor_tensor(
                        out=o_[:], in0=h2[:, j, :], scalar=fin_s[:, j:j + 1], in1=rsh[:], op0=ALU.mult, op1=ALU.mult),
                        reads=[h2, rsh, fin_s], writes=[o_])
                    S.dma("sp", outT.t[b, j * 128:(j + 1) * 128, t0:t0 + 512], o_[:], reads=[o_], writes=[outT], is_out=True)
            S.barrier()

    for b in range(nb):
        if "B" in stages:
            stage_B(b)
        if "C" in stages:
            stage_C(b)
        if "D" in stages:
            stage_D1(b)
            stage_D2(b)
        if "E" in stages:
            stage_E(b)
    if "E" in stages:
        stage_E2()
    if "G" in stages:
        stage_G()
    for b in range(nb):
        if "H" in stages:
            stage_H(b)

    S.finish()
    es.close()
    return nc


def rope_tables():
    quarter = 32
    freqs = (10000.0 ** (-np.arange(quarter, dtype=np.float32) / quarter)).astype(np.float32)
    pos = np.arange(T)
    row, col = (pos // 64).astype(np.float32), (pos % 64).astype(np.float32)
    cos = np.zeros((128, T), np.float32)
    sin = np.zeros((128, T), np.float32)
    for base, p in ((0, row), (64, col)):
        ang = p[None, :] * freqs[:, None]
        cos[base:base + 32] = np.cos(ang); cos[base + 32:base + 64] = np.cos(ang)
        sin[base:base + 32] = -np.sin(ang); sin[base + 32:base + 64] = np.sin(ang)
    perm = np.zeros((128, 128), np.float32)
    for m in range(128):
        partner = m + 32 if (m % 64) < 32 else m - 32
        perm[partner, m] = 1.0
    return cos, sin, perm


def _attn_consts():
    ridx = np.zeros((128, 35, 128), np.int64)
    cidx = np.zeros((128, 35, 128), np.int64)
    mask = np.zeros((128, 35, 128), np.float32)
    key = np.arange(128)[:, None]
    q = np.arange(128)[None, :]
    for case, rp in enumerate((0, 1, 7, 14, 15)):
        r0 = 2 * rp
        start = min(max(r0 - 4, 0), 22)
        qr = r0 + q // 64
        qc = q % 64
        rs = np.clip(qr - 4, 0, 24)
        cs = np.clip(qc - 8, 0, 48)
        for j in range(5):
            kr = start + 2 * j + key // 64
            kc = key % 64
            valid = (kr >= rs) & (kr < rs + 8) & (kc >= cs) & (kc < cs + 16)
            ridx[:, case * 7 + j, :] = np.clip(kr - qr + 7, 0, 14)
            cidx[:, case * 7 + j, :] = np.clip(kc - qc + 15, 0, 30)
            mask[:, case * 7 + j, :] = np.where(valid, 0.0, NEG)
    valid_all = mask == 0.0

    def gather(rpb):
        g = rpb[:, ridx, cidx]
        g = np.where(valid_all[None], g, 0.0).astype(np.float32)
        for case in range(5):
            g[:, :, case * 7 + 5:case * 7 + 7, :] = 0.0
        return np.ascontiguousarray(g)
    for case in range(5):
        mask[:, case * 7 + 5:case * 7 + 7, :] = 0.0
    s_ = np.arange(128)[:, None]
    l_ = np.arange(128)[None, :]
    tri = np.stack([(s_ <= l_), (s_ >= l_)]).astype(np.float32)
    m2 = np.stack([np.where(l_ >= s_, 0.0, NEG), np.where(l_ <= s_, 0.0, NEG)]).astype(np.float32)
    mrep = np.ascontiguousarray(np.tile(m2, (1, 1, 4)))
    return {"rpb_idx": gather, "amask": mask, "tri": tri, "mrep": mrep}


CONSTS = _attn_consts()


def fm_cols(v):
    return np.ascontiguousarray(v.reshape(-1, 128).T)


def make_in_maps(inp, nb=NB, cores=8):
    cos, sin, perm = rope_tables()
    maps = []
    for c in range(cores):
        bs = [c * nb + i for i in range(nb)]
        m = {}
        m["xT"] = np.ascontiguousarray(np.transpose(inp["x"][bs], (0, 2, 1)))
        m["ctxT"] = np.ascontiguousarray(np.transpose(inp["ctx"][bs], (0, 2, 1)))
        conds = [inp["c"][bs[0]], inp["c"][bs[-1]], inp["c_ctx"]]
        m["cT"] = np.ascontiguousarray(np.stack([fm_cols(v) for v in conds], axis=-1))
        m["w_ada"] = inp["w_ada"][0]
        m["b_adaT"] = fm_cols(inp["b_ada"][0])
        m["nmixT"] = fm_cols(inp["norm_mix_w"][0])
        m["nffnT"] = fm_cols(inp["norm_ffn_w"][0])
        m["finT"] = fm_cols(inp["final_norm_w"])
        m["w_in"] = inp["w_in"][0]
        m["cos_t"] = cos; m["sin_t"] = sin; m["permT"] = perm
        m["dtb_bc"] = np.ascontiguousarray(np.broadcast_to(inp["dt_bias"][0].reshape(1, 32), (128, 32)))
        m["ident_f"] = np.eye(128, dtype=np.float32)
        m["rpbG"] = CONSTS["rpb_idx"](inp["rpb"][0])
        m["amask"] = CONSTS["amask"]
        m["tri_in"] = CONSTS["tri"]
        m["mrep_in"] = CONSTS["mrep"]
        m["alog_bc"] = np.ascontiguousarray(np.broadcast_to(inp["a_log"][0].reshape(1, 32), (128, 32)))
        m["dskip_bc"] = np.ascontiguousarray(np.broadcast_to(np.repeat(inp["d_skip"][0], 64).reshape(1, 1024), (128, 1024)))
        m["ssmw_bc"] = np.ascontiguousarray(np.broadcast_to(inp["ssm_norm_w"][0].reshape(1, 1024), (128, 1024)))
        m["convw"] = np.ascontiguousarray(inp["conv_w"][0].reshape(5, 12, 128).transpose(2, 1, 0))
        m["convb"] = fm_cols(inp["conv_b"][0])
        if "w_out" in inp:
            m["w_out"] = inp["w_out"][0]
            m["w_rT"] = np.ascontiguousarray(inp["w_router"][0].reshape(KC, 128, NE).transpose(1, 0, 2))
            sr = np.zeros((NE, NE, 128), np.float32)
            for e_ in range(NE):
                sr[e_, e_, :] = 1.0
            m["selrow_in"] = sr
            m["iota_in"] = np.ascontiguousarray(np.broadcast_to(np.arange(256, dtype=np.float32)[None], (128, 256)))
            m["jcol_in"] = np.stack([np.arange(128), np.arange(128) + 128], axis=1).astype(np.float32)
            tk = np.zeros((128, 16, 2), np.float32)
            tk[:, :, 0] = np.arange(16)[None, :]
            tk[:, :, 1] = np.arange(128)[:, None]
            m["tokid_in"] = tk
        if "w_gate" in inp:
            m["w_gate"] = inp["w_gate"][0]
            m["w_up"] = inp["w_up"][0]
            m["w_down"] = inp["w_down"][0]
        maps.append(m)
    return maps


def kernel(**inp):
    inp = {k: np.asarray(v) for k, v in inp.items()}
    nc = build()
    maps = make_in_maps(inp)
    res = run_bass_kernel_spmd(nc, maps, core_ids=list(range(8)))
    out = np.zeros((16, T, D), np.float32)
    for c in range(8):
        o = res.results[c]["outT"]
        for i in range(NB):
            out[c * NB + i] = o[i].T
    return out
```
